# Optimizing a Trainium2 kernel written in Bass

```python
import jax
import jax.numpy as jnp
from jax import lax
import numpy as np

D_MODEL = 1024
BATCH = 8
SEQ = 4096
DEPTH = 2

CTX_LEN = 256
GRID_W = 64
HEAD_DIM = 64
NORM_EPS = 1e-6
ROPE_BASE = 10000.0
NEG_INF = -1e30
Q_BLOCK = 128

MLA_HEADS = 4
MLA_Q_RANK = 192
MLA_KV_RANK = 128
MLA_NOPE = 64
MLA_ROPE = 32
MLA_V = 64

SWA_Q_HEADS = 8
SWA_KV_HEADS = 2
SWA_WINDOW = 128
SWA_BLOCK = 128

RET_HEADS = 4
RET_DK = 64
RET_DV = 64
RET_CHUNK = 128

D_FF = 2816
N_EXPERTS = 8
TOP_K = 2
D_FF_EXPERT = 3584

IN_SIZES = (MLA_Q_RANK, MLA_KV_RANK, MLA_ROPE,
            SWA_Q_HEADS * HEAD_DIM, SWA_KV_HEADS * HEAD_DIM, SWA_KV_HEADS * HEAD_DIM,
            RET_HEADS * RET_DK, RET_HEADS * RET_DK, RET_HEADS * RET_DV, RET_HEADS * RET_DV)
IN_DIM = sum(IN_SIZES)
MIX_DIM = MLA_HEADS * MLA_V + SWA_Q_HEADS * HEAD_DIM + RET_HEADS * RET_DV
N_DENSE = (DEPTH + 1) // 2
N_MOE = DEPTH // 2

kernel_name = 'hybrid_mla_swa_retention_moe_dit'


def _rms(x):
    xf = x.astype(jnp.float32)
    return xf * lax.rsqrt(jnp.mean(xf * xf, axis=-1, keepdims=True) + NORM_EPS)


def rms_norm(x, g):
    return (_rms(x) * g.astype(jnp.float32)).astype(x.dtype)


def modulate(h, shift, scale):
    return h * (1.0 + scale) + shift


def rope_angles(pos, dim):
    inv = ROPE_BASE ** (-jnp.arange(0, dim, 2, dtype=jnp.float32) / dim)
    ang = pos.astype(jnp.float32)[:, None] * inv[None, :]
    ang = jnp.concatenate([ang, ang], axis=-1)
    return jnp.cos(ang), jnp.sin(ang)


def apply_rope(x, cos, sin):
    x1, x2 = jnp.split(x, 2, axis=-1)
    rot = jnp.concatenate([-x2, x1], axis=-1)
    return (x * cos[:, None, :] + rot * sin[:, None, :]).astype(x.dtype)


def axial_rope(x, rows, cols):
    half = x.shape[-1] // 2
    cr, sr = rope_angles(rows, half)
    cc, sc = rope_angles(cols, half)
    return jnp.concatenate([apply_rope(x[..., :half], cr, sr),
                            apply_rope(x[..., half:], cc, sc)], axis=-1)


def split_proj(p):
    cuts = [int(v) for v in np.cumsum(IN_SIZES)[:-1]]
    return jnp.split(p, cuts, axis=-1)


def flip_seq(t):
    return t[:, ::-1]


def mla_queries(cq, q_norm, w_uq, rows, cols):
    B, L, _ = cq.shape
    q = (rms_norm(cq, q_norm) @ w_uq).reshape(B, L, MLA_HEADS, MLA_NOPE + MLA_ROPE)
    q_pe = q[..., MLA_NOPE:]
    if rows is not None:
        q_pe = axial_rope(q_pe, rows, cols)
    return jnp.concatenate([q[..., :MLA_NOPE], q_pe], axis=-1)


def mla_keys_values(ckv, k_pe, kv_norm, w_ukv, rows, cols):
    B, L, _ = ckv.shape
    kv = (rms_norm(ckv, kv_norm) @ w_ukv).reshape(B, L, MLA_HEADS, MLA_NOPE + MLA_V)
    k_pe = k_pe[:, :, None, :]
    if rows is not None:
        k_pe = axial_rope(k_pe, rows, cols)
    k = jnp.concatenate([kv[..., :MLA_NOPE],
                         jnp.broadcast_to(k_pe, (B, L, MLA_HEADS, MLA_ROPE))], axis=-1)
    return k, kv[..., MLA_NOPE:]


def block_attention(q, k, v, scale):
    B, L, H, d = q.shape
    nb = L // Q_BLOCK
    q_blocks = q.reshape(B, nb, Q_BLOCK, H, d).transpose(1, 0, 2, 3, 4)

    def one_block(q_blk):
        s = jnp.einsum('bqhd,bkhd->bhqk', q_blk, k, preferred_element_type=jnp.float32) * scale
        p = jax.nn.softmax(s, axis=-1).astype(v.dtype)
        return jnp.einsum('bhqk,bkhd->bqhd', p, v)

    o = lax.map(one_block, q_blocks)
    return o.transpose(1, 0, 2, 3, 4).reshape(B, L, -1)


def sink_softmax(s, sink_g):
    sk = jnp.broadcast_to(sink_g.astype(jnp.float32)[None, :, :, None, None], s.shape[:-1] + (1,))
    return jax.nn.softmax(jnp.concatenate([s, sk], axis=-1), axis=-1)[..., :-1]


def banded_sink_attention(q, k, v, k_ctx, v_ctx, sink):
    B, L, Hq, d = q.shape
    Hkv = k.shape[2]
    G = Hq // Hkv
    W = SWA_BLOCK
    nb = L // W
    scale = d ** -0.5
    pad = ((0, 0), (W, W), (0, 0), (0, 0))
    k_pad = jnp.pad(k, pad)
    v_pad = jnp.pad(v, pad)
    q_blocks = q.reshape(B, nb, W, Hkv, G, d).transpose(1, 0, 2, 3, 4, 5)
    offs = jnp.arange(3 * W) - W
    rel = offs[None, :] - jnp.arange(W)[:, None]
    in_band = jnp.abs(rel) <= SWA_WINDOW
    sink_g = sink.reshape(Hkv, G)

    def one_block(args):
        i, q_blk = args
        start = i * W
        k_blk = lax.dynamic_slice_in_dim(k_pad, start, 3 * W, axis=1)
        v_blk = lax.dynamic_slice_in_dim(v_pad, start, 3 * W, axis=1)
        key_t = start + offs
        valid = in_band & ((key_t >= 0) & (key_t < L))[None, :]
        s_band = jnp.einsum('bqhgd,bkhd->bhgqk', q_blk, k_blk,
                            preferred_element_type=jnp.float32) * scale
        s_band = jnp.where(valid, s_band, NEG_INF)
        s_ctx = jnp.einsum('bqhgd,bkhd->bhgqk', q_blk, k_ctx,
                           preferred_element_type=jnp.float32) * scale
        p = sink_softmax(jnp.concatenate([s_band, s_ctx], axis=-1), sink_g)
        p_band = p[..., :3 * W].astype(v.dtype)
        p_ctx = p[..., 3 * W:].astype(v.dtype)
        return (jnp.einsum('bhgqk,bkhd->bqhgd', p_band, v_blk)
                + jnp.einsum('bhgqk,bkhd->bqhgd', p_ctx, v_ctx))

    o = lax.map(one_block, (jnp.arange(nb), q_blocks))
    return o.transpose(1, 0, 2, 3, 4, 5).reshape(B, L, Hq * d)


def full_sink_attention(q, k, v, sink):
    B, L, Hq, d = q.shape
    Hkv = k.shape[2]
    G = Hq // Hkv
    qg = q.reshape(B, L, Hkv, G, d)
    s = jnp.einsum('bqhgd,bkhd->bhgqk', qg, k, preferred_element_type=jnp.float32) * d ** -0.5
    p = sink_softmax(s, sink.reshape(Hkv, G)).astype(v.dtype)
    return jnp.einsum('bhgqk,bkhd->bqhgd', p, v).reshape(B, L, Hq * d)


def retention_scan(q, k, v, gamma, state0):
    B, L, H, _ = q.shape
    dv = v.shape[-1]
    C = RET_CHUNK
    n = L // C
    log_g = jnp.log(gamma)[:, None]
    idx = jnp.arange(C, dtype=jnp.float32)
    diff = idx[:, None] - idx[None, :]
    intra = jnp.where(diff >= 0, jnp.exp(log_g[:, :, None] * jnp.maximum(diff, 0.0)), 0.0)
    q_dec = jnp.exp(log_g * (idx + 1.0))[:, :, None]
    k_dec = jnp.exp(log_g * (C - 1.0 - idx))[:, :, None]
    chunk_dec = jnp.exp(log_g * C)[:, :, None]

    def to_chunks(t):
        return t.astype(jnp.float32).reshape(B, n, C, H, -1).transpose(1, 0, 3, 2, 4)

    def step(state, inp):
        qi, ki, vi = inp
        att = jnp.einsum('bhqd,bhkd->bhqk', qi, ki) * intra
        out = (jnp.einsum('bhqk,bhkv->bhqv', att, vi)
               + jnp.einsum('bhqd,bhdv->bhqv', qi * q_dec, state))
        state = chunk_dec * state + jnp.einsum('bhkd,bhkv->bhdv', ki * k_dec, vi)
        return state, out

    state, out = lax.scan(step, state0, (to_chunks(q), to_chunks(k), to_chunks(v)))
    return out.transpose(1, 0, 3, 2, 4).reshape(B, L, H, dv), state


def retention_output(o, g):
    B, L = g.shape[:2]
    y = _rms(o).reshape(B, L, -1)
    return (y * jax.nn.silu(g.astype(jnp.float32))).astype(g.dtype)


def hybrid_mixer(h, hc, w_in, q_norm, w_uq, kv_norm, w_ukv, sink, decay_f, decay_b, w_out,
                 need_ctx_out):
    B, L, _ = h.shape
    Lc = hc.shape[1]
    n_rows = L // GRID_W
    t = jnp.arange(n_rows * GRID_W)
    rows, cols = t // GRID_W, t % GRID_W
    cq, ckv, kpe, sq, sk, sv, rq, rk, rv, rg = split_proj(h @ w_in)
    cq_x, ckv_x, kpe_x, sq_x, sk_x, sv_x, rq_x, rk_x, rv_x, rg_x = split_proj(hc @ w_in)

    mla_scale = (MLA_NOPE + MLA_ROPE) ** -0.5
    k_a, v_a = mla_keys_values(ckv, kpe, kv_norm, w_ukv, rows, cols)
    k_a_x, v_a_x = mla_keys_values(ckv_x, kpe_x, kv_norm, w_ukv, None, None)
    q_a = mla_queries(cq, q_norm, w_uq, rows, cols)
    o_a = block_attention(q_a, jnp.concatenate([k_a, k_a_x], axis=1),
                          jnp.concatenate([v_a, v_a_x], axis=1), mla_scale)

    q_b = axial_rope(sq.reshape(B, L, SWA_Q_HEADS, HEAD_DIM), rows, cols)
    k_b = axial_rope(sk.reshape(B, L, SWA_KV_HEADS, HEAD_DIM), rows, cols)
    v_b = sv.reshape(B, L, SWA_KV_HEADS, HEAD_DIM)
    k_b_x = sk_x.reshape(B, Lc, SWA_KV_HEADS, HEAD_DIM)
    v_b_x = sv_x.reshape(B, Lc, SWA_KV_HEADS, HEAD_DIM)
    o_b = banded_sink_attention(q_b, k_b, v_b, k_b_x, v_b_x, sink)

    gamma_f = jax.nn.sigmoid(decay_f.astype(jnp.float32))
    gamma_b = jax.nn.sigmoid(decay_b.astype(jnp.float32))
    cos, sin = rope_angles(t, RET_DK)
    k_scale = RET_DK ** -0.5
    q_c = apply_rope(rq.reshape(B, L, RET_HEADS, RET_DK), cos, sin)
    k_c = apply_rope(rk.reshape(B, L, RET_HEADS, RET_DK), cos, sin) * k_scale
    v_c = rv.reshape(B, L, RET_HEADS, RET_DV)
    q_c_x = rq_x.reshape(B, Lc, RET_HEADS, RET_DK)
    k_c_x = rk_x.reshape(B, Lc, RET_HEADS, RET_DK) * k_scale
    v_c_x = rv_x.reshape(B, Lc, RET_HEADS, RET_DV)
    zero_state = jnp.zeros((B, RET_HEADS, RET_DK, RET_DV), jnp.float32)
    oc_f, s_f = retention_scan(q_c_x, k_c_x, v_c_x, gamma_f, zero_state)
    oc_b, s_b = retention_scan(flip_seq(q_c_x), flip_seq(k_c_x), flip_seq(v_c_x), gamma_b, zero_state)
    o_f, _ = retention_scan(q_c, k_c, v_c, gamma_f, s_f)
    o_bk, _ = retention_scan(flip_seq(q_c), flip_seq(k_c), flip_seq(v_c), gamma_b, s_b)
    o_c = retention_output(o_f + flip_seq(o_bk), rg)

    y = jnp.concatenate([o_a, o_b, o_c], axis=-1) @ w_out
    if not need_ctx_out:
        return y, None

    q_a_x = mla_queries(cq_x, q_norm, w_uq, None, None)
    oc_a = block_attention(q_a_x, k_a_x, v_a_x, mla_scale)
    oc_bw = full_sink_attention(sq_x.reshape(B, Lc, SWA_Q_HEADS, HEAD_DIM), k_b_x, v_b_x, sink)
    oc_c = retention_output(oc_f + flip_seq(oc_b), rg_x)
    yc = jnp.concatenate([oc_a, oc_bw, oc_c], axis=-1) @ w_out
    return y, yc


def swiglu(h, w_gate, w_up, w_down):
    return (jax.nn.silu(h @ w_gate) * (h @ w_up)) @ w_down


def moe_swiglu(h, router, w_gate, w_up, w_down):
    logits = (h @ router).astype(jnp.float32)
    top_v, top_i = lax.top_k(logits, TOP_K)
    top_w = jax.nn.softmax(top_v, axis=-1)
    gates = jnp.sum(jax.nn.one_hot(top_i, N_EXPERTS, dtype=jnp.float32) * top_w[..., None], axis=-2)
    out = jnp.zeros_like(h)
    for e in range(N_EXPERTS):
        out = out + gates[..., e:e + 1].astype(h.dtype) * swiglu(h, w_gate[e], w_up[e], w_down[e])
    return out


def channel_mixer(h, layer, ffn_w_gate, ffn_w_up, ffn_w_down, moe_router, moe_w_gate, moe_w_up,
                  moe_w_down):
    i = layer // 2
    if layer % 2 == 0:
        return swiglu(h, ffn_w_gate[i], ffn_w_up[i], ffn_w_down[i])
    return moe_swiglu(h, moe_router[i], moe_w_gate[i], moe_w_up[i], moe_w_down[i])


def setup_inputs(seed: int = 0) -> dict:
    key = jax.random.key(seed)
    keys = iter(jax.random.split(key, 32))
    f32 = jnp.float32

    def normal(shape, scale):
        return jax.random.normal(next(keys), shape, f32) * scale

    def gain(shape):
        return 1.0 + normal(shape, 0.02)

    gamma0 = 1.0 - 2.0 ** (-5.0 - jnp.arange(RET_HEADS, dtype=f32))
    decay_logit0 = jnp.log(gamma0) - jnp.log1p(-gamma0)
    return {
        'x': normal((BATCH, SEQ, D_MODEL), 1.0),
        'c': normal((BATCH, D_MODEL), 1.0),
        'ctx': normal((BATCH, CTX_LEN, D_MODEL), 1.0),
        'c_ctx': normal((D_MODEL,), 1.0),
        'w_mod': normal((DEPTH, D_MODEL, 6 * D_MODEL), 0.5 * D_MODEL ** -0.5),
        'b_mod': normal((DEPTH, 6 * D_MODEL), 0.01),
        'norm1_g': gain((DEPTH, D_MODEL)),
        'norm2_g': gain((DEPTH, D_MODEL)),
        'w_in': normal((DEPTH, D_MODEL, IN_DIM), D_MODEL ** -0.5),
        'mla_q_norm': gain((DEPTH, MLA_Q_RANK)),
        'mla_w_uq': normal((DEPTH, MLA_Q_RANK, MLA_HEADS * (MLA_NOPE + MLA_ROPE)), MLA_Q_RANK ** -0.5),
        'mla_kv_norm': gain((DEPTH, MLA_KV_RANK)),
        'mla_w_ukv': normal((DEPTH, MLA_KV_RANK, MLA_HEADS * (MLA_NOPE + MLA_V)), MLA_KV_RANK ** -0.5),
        'swa_sink': normal((DEPTH, SWA_Q_HEADS), 0.5),
        'ret_decay_fwd': decay_logit0 + normal((DEPTH, RET_HEADS), 0.05),
        'ret_decay_bwd': decay_logit0 + normal((DEPTH, RET_HEADS), 0.05),
        'w_out': normal((DEPTH, MIX_DIM, D_MODEL), MIX_DIM ** -0.5),
        'ffn_w_gate': normal((N_DENSE, D_MODEL, D_FF), D_MODEL ** -0.5),
        'ffn_w_up': normal((N_DENSE, D_MODEL, D_FF), D_MODEL ** -0.5),
        'ffn_w_down': normal((N_DENSE, D_FF, D_MODEL), D_FF ** -0.5),
        'moe_router': normal((N_MOE, D_MODEL, N_EXPERTS), D_MODEL ** -0.5),
        'moe_w_gate': normal((N_MOE, N_EXPERTS, D_MODEL, D_FF_EXPERT), D_MODEL ** -0.5),
        'moe_w_up': normal((N_MOE, N_EXPERTS, D_MODEL, D_FF_EXPERT), D_MODEL ** -0.5),
        'moe_w_down': normal((N_MOE, N_EXPERTS, D_FF_EXPERT, D_MODEL), D_FF_EXPERT ** -0.5),
        'final_norm_g': gain((D_MODEL,)),
    }


def reference(x, c, ctx, c_ctx, w_mod, b_mod, norm1_g, norm2_g, w_in, mla_q_norm, mla_w_uq,
              mla_kv_norm, mla_w_ukv, swa_sink, ret_decay_fwd, ret_decay_bwd, w_out,
              ffn_w_gate, ffn_w_up, ffn_w_down, moe_router, moe_w_gate, moe_w_up, moe_w_down,
              final_norm_g):
    xc = ctx
    cond_lat = jax.nn.silu(c)
    cond_ctx = jax.nn.silu(c_ctx)[None]
    for layer in range(DEPTH):
        last = layer == DEPTH - 1
        mod = (cond_lat @ w_mod[layer] + b_mod[layer])[:, None, :]
        mod_x = (cond_ctx @ w_mod[layer] + b_mod[layer])[:, None, :]
        sh1, sc1, g1, sh2, sc2, g2 = jnp.split(mod, 6, axis=-1)
        sh1x, sc1x, g1x, sh2x, sc2x, g2x = jnp.split(mod_x, 6, axis=-1)

        h = modulate(rms_norm(x, norm1_g[layer]), sh1, sc1)
        hc = modulate(rms_norm(xc, norm1_g[layer]), sh1x, sc1x)
        y, yc = hybrid_mixer(h, hc, w_in[layer], mla_q_norm[layer], mla_w_uq[layer],
                             mla_kv_norm[layer], mla_w_ukv[layer], swa_sink[layer],
                             ret_decay_fwd[layer], ret_decay_bwd[layer], w_out[layer],
                             not last)
        x = x + g1 * y
        h = modulate(rms_norm(x, norm2_g[layer]), sh2, sc2)
        x = x + g2 * channel_mixer(h, layer, ffn_w_gate, ffn_w_up, ffn_w_down, moe_router,
                                   moe_w_gate, moe_w_up, moe_w_down)
        if not last:
            xc = xc + g1x * yc
            hcf = modulate(rms_norm(xc, norm2_g[layer]), sh2x, sc2x)
            xc = xc + g2x * channel_mixer(hcf, layer, ffn_w_gate, ffn_w_up, ffn_w_down, moe_router,
                                          moe_w_gate, moe_w_up, moe_w_down)
    return rms_norm(x, final_norm_g)
```

```python
import os
import contextlib
import numpy as np
import concourse.bass as bass
import concourse.mybir as mybir
from concourse.bass_utils import run_bass_kernel_spmd

F32 = mybir.dt.float32
BF16 = mybir.dt.bfloat16
AF = mybir.ActivationFunctionType
ALU = mybir.AluOpType
AX = mybir.AxisListType

D = 1024
L = 4096
LC = 256
NT = 34
NLT = 32
DEPTH = 2
IN_DIM = 2144
D_FF = 2816
NE = 8
D_FFE = 3584
EPS = 1e-6


class Buf:
    __slots__ = ("name", "w", "r", "excl")

    def __init__(self, name="", excl=False):
        self.name = name
        self.w = None
        self.r = {}
        self.excl = excl


class _Rec:
    def __init__(self):
        self.call = None

    def __getattr__(self, name):
        def f(*a, **kw):
            self.call = (name, a, kw)
            return self
        return f


def _bind(fn):
    r = _Rec()
    fn(r)
    name, a, kw = r.call
    return lambda eng: getattr(eng, name)(*a, **kw)


class KS:
    ENG = ("pe", "act", "dve", "pool", "sp")

    def __init__(self, nc, st, n_dma_sems=16):
        self.nc = nc
        self.st = st
        self.prog = {e: [] for e in self.ENG}
        self.sems = {}
        self.cnt = {}
        self.seen = {e: {} for e in self.ENG}
        for e in self.ENG:
            self._mksem("E_" + e)
        self.dma_q = {}
        for q in ("sp", "pool", "act"):
            keys = []
            for i in range(n_dma_sems if q == "sp" else (8 if q == "act" else 4)):
                k = "D_%s%d" % (q, i)
                self._mksem(k)
                keys.append(k)
            self.dma_q[q] = [keys, 0]
        self.n_instr = 0
        self.maxi = int(os.environ.get("DBG_MAXI", 10 ** 9))
        self.scr = st.enter_context(nc.sbuf_tensor("ks_scr", [128, 8], F32))
        self.prog["pool"].append(([], lambda eng: eng.memset(self.scr[:], 0.0), "E_pool", 1))
        self.cnt["E_pool"] += 1

    def _mksem(self, key):
        h = self.st.enter_context(self.nc.semaphore(key))
        self.sems[key] = h
        self.cnt[key] = 0

    def _dummy(self, e):
        key = "E_" + e
        self.cnt[key] += 1
        scr = self.scr
        col = {"act": 0, "dve": 1, "pool": 2}[e]
        if e == "act":
            fn = lambda eng: eng.copy(out=scr[:, col:col + 1], in_=scr[:, col + 4:col + 5])
        else:
            fn = lambda eng: eng.tensor_copy(out=scr[:, col:col + 1], in_=scr[:, col + 4:col + 5])
        self.prog[e].append(([], fn, key, 1))
        self.n_instr += 1

    def _deps(self, e, reads, writes):
        need = {}

        def add(ev):
            if ev is None:
                return
            k, v = ev
            if need.get(k, 0) < v:
                need[k] = v
        for b in reads:
            add(b.w)
            if b.excl:
                for k, v in b.r.items():
                    if k != "E_" + e:
                        add((k, v))
        for b in writes:
            add(b.w)
            for k, v in b.r.items():
                add((k, v))
        out = []
        own = "E_" + e
        for k, v in need.items():
            if k == own and (e == "pe" or e == "sp"):
                continue
            if self.seen[e].get(k, 0) >= v:
                continue
            self.seen[e][k] = v
            out.append((k, v))
        return out

    def op(self, e, fn, reads=(), writes=()):
        if self.n_instr >= self.maxi:
            return
        waits = self._deps(e, reads, writes)
        key = "E_" + e
        self.cnt[key] += 1
        val = self.cnt[key]
        self.prog[e].append((waits, _bind(fn), key, 1))
        for b in reads:
            if b.r.get(key, 0) < val:
                b.r[key] = val
        for b in writes:
            b.w = (key, val)
            b.r = {}
        self.n_instr += 1

    def dma(self, q, out, in_, reads=(), writes=(), **kw):
        if self.n_instr >= self.maxi:
            return
        keys, idx = self.dma_q[q]
        key = keys[idx % len(keys)]
        self.dma_q[q][1] = idx + 1
        waits = self._deps(q, reads, writes)
        prev = self.cnt[key]
        if prev > 0 and self.seen[q].get(key, 0) < prev:
            self.seen[q][key] = prev
            waits.append((key, prev))
        self.cnt[key] += 16
        val = self.cnt[key]

        def fn(eng, out=out, in_=in_, kw=kw):
            return eng.dma_start(out=out, in_=in_, **kw)
        self.prog[q].append((waits, fn, key, 16))
        for b in reads:
            if b.r.get(key, 0) < val:
                b.r[key] = val
        for b in writes:
            b.w = (key, val)
            b.r = {}
        self.n_instr += 1

    def barrier(self, engines=None):
        engines = engines or self.ENG
        snap = dict(self.cnt)
        for e in engines:
            waits = []
            for k, v in snap.items():
                if v == 0:
                    continue
                if k == "E_" + e and e in ("pe", "sp"):
                    continue
                if self.seen[e].get(k, 0) >= v:
                    continue
                self.seen[e][k] = v
                waits.append((k, v))
            if waits:
                self.prog[e].append((waits, None, None, 0))

    def emit(self):
        nc = self.nc
        ks = self
        with nc.Block() as block:
            def run(e):
                def body(eng):
                    for waits, fn, key, inc in ks.prog[e]:
                        for k, v in waits:
                            eng.wait_ge(ks.sems[k], v)
                        if fn is not None:
                            fn(eng).then_inc(ks.sems[key], inc)
                return body
            block.sync(run("sp"))
            block.tensor(run("pe"))
            block.scalar(run("act"))
            block.vector(run("dve"))
            block.gpsimd(run("pool"))


def run_pipelined(make_gen, items, depth):
    items = list(items)
    active = []
    nxt = 0
    while nxt < len(items) or active:
        if nxt < len(items) and len(active) < depth:
            active.append(make_gen(items[nxt]))
            nxt += 1
        for g in list(active):
            try:
                next(g)
            except StopIteration:
                active.remove(g)


class Ring:
    def __init__(self, alloc, name, n, shape, dt):
        self.tiles = [alloc("%s%d" % (name, i), shape, dt) for i in range(n)]
        excl = getattr(alloc, "__name__", "") == "ps"
        self.bufs = [Buf("%s%d" % (name, i), excl) for i in range(n)]
        self.i = 0

    def next(self):
        j = self.i % len(self.tiles)
        self.i += 1
        return self.tiles[j], self.bufs[j]


def _rope_cs(pos, dim):
    inv = (10000.0 ** (-np.arange(0, dim, 2, dtype=np.float32) / np.float32(dim))).astype(np.float32)
    ang = pos.astype(np.float32)[:, None] * inv[None, :]
    ang = np.concatenate([ang, ang], axis=-1).astype(np.float32)
    c = np.cos(ang).astype(np.float32)
    s = np.sin(ang).astype(np.float32)
    h = dim // 2
    s = np.concatenate([-s[:, :h], s[:, h:]], axis=-1)
    return c, s


def _consts():
    t = np.arange(L)
    rows, cols = t // 64, t % 64
    out = {}
    for nm, dim in (("ropeA", 32), ("ropeB", 64)):
        h = dim // 2
        cr, sr = _rope_cs(rows, h)
        cc, sc = _rope_cs(cols, h)
        c = np.concatenate([cr, cc], -1)
        s = np.concatenate([sr, sc], -1)
        out[nm + "_c"] = np.ascontiguousarray(c.reshape(NLT, 128, dim).transpose(1, 0, 2))
        out[nm + "_s"] = np.ascontiguousarray(s.reshape(NLT, 128, dim).transpose(1, 0, 2))
    c, s = _rope_cs(t, 64)
    ks_ = np.float32(64 ** -0.5)
    c2 = np.stack([c, c * ks_], 1)
    s2 = np.stack([s, s * ks_], 1)
    out["ropeC_c"] = np.ascontiguousarray(c2.reshape(NLT, 128, 2, 64).transpose(1, 0, 2, 3))
    out["ropeC_s"] = np.ascontiguousarray(s2.reshape(NLT, 128, 2, 64).transpose(1, 0, 2, 3))
    out["ident"] = np.eye(128, dtype=np.float32)
    k = np.arange(128)[:, None]
    q = np.arange(128)[None, :]
    NEG = np.float32(-30000.0)
    out["mask_prev"] = np.where(k >= q, 0.0, NEG).astype(np.float32)
    out["mask_next"] = np.where(k <= q, 0.0, NEG).astype(np.float32)
    out["ret_d1"] = np.maximum(q - k, 0).astype(np.float32)
    out["ret_i1"] = (q >= k).astype(np.float32)
    out["ret_d2"] = np.maximum(k - q, 0).astype(np.float32)
    out["ret_i2"] = (k >= q).astype(np.float32)
    out["ret_j1"] = np.broadcast_to((np.arange(128) + 1.0)[None, :], (128, 128)).astype(np.float32).copy()
    out["ret_j2"] = np.broadcast_to((128.0 - np.arange(128))[None, :], (128, 128)).astype(np.float32).copy()
    pc = np.zeros((128, 4), np.float32)
    pc[:, 0] = 127.0 - np.arange(128)
    pc[:, 1] = np.arange(128)
    pc[:, 2] = np.arange(128) + 1.0
    pc[:, 3] = 128.0 - np.arange(128)
    out["ret_pc"] = pc
    return out


CONST_SHAPES = {k: v.shape for k, v in _consts().items()}

W_SHAPES = {
    "w_mod": (DEPTH, D, 6 * D), "b_mod": (DEPTH, 6 * D), "norm1_g": (DEPTH, D), "norm2_g": (DEPTH, D),
    "w_in": (DEPTH, D, IN_DIM), "mla_q_norm": (DEPTH, 192), "mla_w_uq": (DEPTH, 192, 384),
    "mla_kv_norm": (DEPTH, 128), "mla_w_ukv": (DEPTH, 128, 512), "swa_sink": (DEPTH, 8),
    "ret_decay_fwd": (DEPTH, 4), "ret_decay_bwd": (DEPTH, 4), "w_out": (DEPTH, D, D),
    "ffn_w_gate": (1, D, D_FF), "ffn_w_up": (1, D, D_FF), "ffn_w_down": (1, D_FF, D),
    "moe_router": (1, D, NE), "moe_w_gate": (1, NE, D, D_FFE), "moe_w_up": (1, NE, D, D_FFE),
    "moe_w_down": (1, NE, D_FFE, D), "final_norm_g": (D,),
}


def build(stop_after=None, debug=False, n_layers=DEPTH):
    nc = bass.Bass("TRN2", target_bir_lowering=False)
    dkind = "ExternalOutput" if debug else "Internal"

    def din(name, shape):
        return nc.dram_tensor(name, list(shape), F32, kind="ExternalInput").ap()

    x_in = din("x", (L, D))
    ctx_in = din("ctx", (LC, D))
    c2_in = din("c2", (2, D))
    W = {k: din(k, s) for k, s in W_SHAPES.items()}
    C = {k: din(k, s) for k, s in CONST_SHAPES.items()}
    out_d = nc.dram_tensor("out", [L, D], F32, kind="ExternalOutput").ap()
    xres = nc.dram_tensor("xres", [NT * 128, D], F32, kind=dkind).ap()
    modv = nc.dram_tensor("modv", [DEPTH, 2, 6 * D], F32, kind=dkind).ap()
    hT_d = nc.dram_tensor("hT_d", [NT, 128, D], BF16, kind=dkind).ap()
    mixT_d = nc.dram_tensor("mixT_d", [8, 128, NT * 128], BF16, kind=dkind).ap()

    done = [False]

    with contextlib.ExitStack() as st0:
        k = KS(nc, st0)

        uid = [0]

        def mk_alloc(st):
            def sb(name, shape, dt):
                uid[0] += 1
                return st.enter_context(nc.sbuf_tensor("s%d_%s" % (uid[0], name), list(shape), dt))

            def ps(name, shape, dt):
                uid[0] += 1
                return st.enter_context(nc.psum_tensor("p%d_%s" % (uid[0], name), list(shape), dt))
            return sb, ps

        sb0, ps0 = mk_alloc(st0)
        ident_f = sb0("ident_f", [128, 128], F32)
        ident_b = sb0("ident_b", [128, 128], BF16)
        ones_f = sb0("ones_f", [128, 128], F32)
        b_c = Buf("consts")
        k.dma("sp", ident_f[:], C["ident"][:, :], writes=[b_c])
        k.op("dve", lambda e: e.tensor_copy(out=ident_b[:], in_=ident_f[:]), reads=[b_c], writes=[b_c])
        k.op("pool", lambda e: e.memset(ones_f[:], 1.0), writes=[b_c])
        k.barrier()

        def rms_rstd(ss_ap, rstd_ap, n, b_ss, b_r):
            k.op("dve", lambda e: e.tensor_scalar(out=rstd_ap, in0=ss_ap, scalar1=1.0 / n, scalar2=EPS,
                                                  op0=ALU.mult, op1=ALU.add), reads=[b_ss], writes=[b_r])
            k.op("act", lambda e: e.activation(out=rstd_ap, in_=rstd_ap, func=AF.Sqrt), reads=[b_r], writes=[b_r])
            k.op("dve", lambda e: e.reciprocal(out=rstd_ap, in_=rstd_ap), reads=[b_r], writes=[b_r])

        def x_src(layer, t):
            if layer == 0:
                return x_in[t * 128:(t + 1) * 128, :] if t < NLT else ctx_in[(t - NLT) * 128:(t - NLT + 1) * 128, :]
            return xres[t * 128:(t + 1) * 128, :]

        def load_rep(dst, src_row, b):
            k.dma("sp", dst, src_row.broadcast_to([128, src_row.shape[-1]]), writes=[b])

        def rope(eng, dst, src, tc_, ts_, tmp, rd, wr, b_tmp):
            k.op(eng, lambda e: e.tensor_tensor(out=tmp[:, :, :, 0, :], in0=src[:, :, :, 1, :], in1=ts_[:, :, :, 0, :],
                                                op=ALU.mult), reads=rd, writes=[b_tmp])
            k.op(eng, lambda e: e.tensor_tensor(out=tmp[:, :, :, 1, :], in0=src[:, :, :, 0, :], in1=ts_[:, :, :, 1, :],
                                                op=ALU.mult), reads=rd, writes=[b_tmp])
            k.op(eng, lambda e: e.tensor_tensor(out=dst, in0=src, in1=tc_, op=ALU.mult), reads=rd, writes=wr)
            k.op(eng, lambda e: e.tensor_tensor(out=dst, in0=dst, in1=tmp, op=ALU.add), reads=[b_tmp] + wr, writes=wr)

        for layer in range(n_layers):
            last = layer == DEPTH - 1
            ntile = NLT if last else NT

            with contextlib.ExitStack() as st:
                sb, ps = mk_alloc(st)
                c2 = sb("c2", [2, D], F32)
                c2s = sb("c2s", [2, D], F32)
                cT = sb("cT", [128, 8, 2], F32)
                bm = sb("bm", [2, 6 * D], F32)
                mo = sb("mo", [2, 6 * D], F32)
                wm = Ring(sb, "wm", 2, [128, 8, 512], F32)
                pT = ps("p0T", [128, 8, 2], F32)
                pm = Ring(ps, "p0m", 2, [2, 512], F32)
                b_c2, b_cT, b_pT, b_bm, b_mo = Buf(), Buf(), Buf(), Buf(), Buf()
                k.dma("sp", c2[:], c2_in[:, :], writes=[b_c2])
                k.dma("sp", bm[:], W["b_mod"][layer:layer + 1, :].broadcast_to([2, 6 * D]), writes=[b_bm])
                k.op("act", lambda e: e.activation(out=c2s[:], in_=c2[:], func=AF.Silu), reads=[b_c2], writes=[b_c2])
                for kc in range(8):
                    k.op("pe", lambda e, kc=kc: e.transpose(out=pT[:, kc, :], in_=c2s[:, kc * 128:(kc + 1) * 128],
                                                            identity=ident_f[0:2, 0:2]), reads=[b_c2], writes=[b_pT])
                k.op("dve", lambda e: e.tensor_copy(out=cT[:], in_=pT[:]), reads=[b_pT], writes=[b_cT])
                for cc in range(12):
                    wt, wb = wm.next()
                    k.dma("sp", wt[:], W["w_mod"][layer, :, cc * 512:(cc + 1) * 512].rearrange("(c p) n -> p c n", p=128),
                          writes=[wb])
                    pt, pb = pm.next()
                    for kc in range(8):
                        k.op("pe", lambda e, kc=kc, pt=pt, wt=wt: e.matmul(pt[:], lhsT=cT[:, kc, :], rhs=wt[:, kc, :],
                                                                           start=(kc == 0), stop=(kc == 7)),
                             reads=[b_cT, wb], writes=[pb])
                    k.op("dve", lambda e, pt=pt, cc=cc: e.tensor_tensor(out=mo[:, cc * 512:(cc + 1) * 512], in0=pt[:],
                                                                        in1=bm[:, cc * 512:(cc + 1) * 512], op=ALU.add),
                         reads=[pb, b_bm], writes=[b_mo])
                k.dma("sp", modv[layer], mo[:], reads=[b_mo])
                k.barrier()
            if stop_after == ("P0", layer):
                break

            def mod_row(which, idx):
                return modv[layer, which:which + 1, idx * D:(idx + 1) * D]

            def load_affine(sb, name, which, norm_g, i_sc, i_sh):
                A = sb(name + "A", [128, D], F32)
                SH = sb(name + "S", [128, D], F32)
                b = Buf()
                with contextlib.ExitStack() as stg_:
                    G = stg_.enter_context(nc.sbuf_tensor("%s_G%d" % (name, layer), [128, D], F32))
                    load_rep(A[:], mod_row(which, i_sc), b)
                    load_rep(SH[:], mod_row(which, i_sh), b)
                    load_rep(G[:], norm_g[layer:layer + 1, :], b)
                    k.op("dve", lambda e: e.scalar_tensor_tensor(out=A[:], in0=A[:], scalar=1.0, in1=G[:], op0=ALU.add,
                                                                 op1=ALU.mult), reads=[b], writes=[b])
                    k.barrier()
                return A, SH, b

            with contextlib.ExitStack() as st:
                sb, ps = mk_alloc(st)
                AL, SL, b_afl = load_affine(sb, "n1l", 0, W["norm1_g"], 1, 0)
                AC, SC, b_afc = load_affine(sb, "n1c", 1, W["norm1_g"], 1, 0)
                winA = sb("winA", [128, 8, 352], BF16)
                wuq0 = sb("wuq0", [128, 384], BF16)
                wuq1 = sb("wuq1", [64, 384], BF16)
                wukv = sb("wukv", [128, 512], BF16)
                b_w = Buf()
                with contextlib.ExitStack() as stw:
                    sbw, _ = mk_alloc(stw)
                    stg = sbw("stgA", [128, 8, 352], F32)
                    s1 = sbw("stg1", [128, 512], F32)
                    s2 = sbw("stg2", [64, 384], F32)
                    s3 = sbw("stg3", [128, 512], F32)
                    gq = sbw("gq", [128, 2], F32)
                    gkv = sbw("gkv", [128, 1], F32)
                    b_s = Buf()
                    k.dma("sp", stg[:], W["w_in"][layer, :, 0:352].rearrange("(c p) n -> p c n", p=128), writes=[b_s])
                    k.dma("sp", s1[:, 0:384], W["mla_w_uq"][layer, 0:128, :], writes=[b_s])
                    k.dma("sp", s2[:], W["mla_w_uq"][layer, 128:192, :], writes=[b_s])
                    k.dma("sp", s3[:], W["mla_w_ukv"][layer, :, :], writes=[b_s])
                    k.dma("sp", gq[:, 0:1], W["mla_q_norm"][layer, 0:128].rearrange("(p o) -> p o", o=1), writes=[b_s])
                    k.dma("sp", gq[0:64, 1:2], W["mla_q_norm"][layer, 128:192].rearrange("(p o) -> p o", o=1), writes=[b_s])
                    k.dma("sp", gkv[:], W["mla_kv_norm"][layer, :].rearrange("(p o) -> p o", o=1), writes=[b_s])
                    k.op("pool", lambda e: e.tensor_copy(out=winA[:], in_=stg[:]), reads=[b_s], writes=[b_w])
                    k.op("dve", lambda e: e.tensor_scalar(out=wuq0[:], in0=s1[:, 0:384], scalar1=gq[:, 0:1], scalar2=None,
                                                          op0=ALU.mult), reads=[b_s], writes=[b_w])
                    k.op("dve", lambda e: e.tensor_scalar(out=wuq1[:], in0=s2[:], scalar1=gq[0:64, 1:2], scalar2=None,
                                                          op0=ALU.mult), reads=[b_s], writes=[b_w])
                    k.op("dve", lambda e: e.tensor_scalar(out=wukv[:], in0=s3[:], scalar1=gkv[:, 0:1], scalar2=None,
                                                          op0=ALU.mult), reads=[b_s], writes=[b_w])
                    k.barrier()
                rc = sb("ropeAc", [128, NLT, 32], F32)
                rs = sb("ropeAs", [128, NLT, 32], F32)
                b_rt = Buf()
                k.dma("sp", rc[:], C["ropeA_c"][:, :, :], writes=[b_rt])
                k.dma("sp", rs[:], C["ropeA_s"][:, :, :], writes=[b_rt])
                QT = sb("QTa", [96, 4, NT * 128], BF16)
                KT = sb("KTa", [96, 4, NT * 128], BF16)
                VA = sb("VA", [128, NT, 2, 192], BF16)
                b_QT, b_KT, b_VA = Buf(), Buf(), Buf()
                k.op("pool", lambda e: e.memset(VA[:], 0.0), writes=[b_VA])
                k.op("pool", lambda e: e.memset(VA[:, :, :, 64:65], 1.0), writes=[b_VA])
                k.barrier()
                with contextlib.ExitStack() as stp:
                    sbp, psp = mk_alloc(stp)
                    xr = Ring(sbp, "xt", 3, [128, D], F32)
                    junk = sbp("junk", [128, D], BF16)
                    b_junk = Buf()
                    h1r = Ring(sbp, "h1", 2, [128, D], F32)
                    hbr = Ring(sbp, "hb", 3, [128, D], BF16)
                    hTr = Ring(sbp, "hT", 3, [128, 8, 128], BF16)
                    st4 = Ring(sbp, "st4", 3, [128, 8], F32)
                    cnr = Ring(sbp, "cn", 3, [128, 320], BF16)
                    cTr = Ring(sbp, "cTs", 3, [128, 3, 128], BF16)
                    kper = Ring(sbp, "kpe", 3, [128, 32], F32)
                    tmpr = Ring(sbp, "rtmp", 3, [128, 4, 32], F32)
                    qfr = Ring(sbp, "qf", 3, [128, 4, 96], BF16)
                    qsr = Ring(sbp, "qs", 3, [128, 384], F32)
                    kfr = Ring(sbp, "kf", 3, [128, 4, 96], BF16)
                    pTr = Ring(psp, "pT", 2, [128, 8, 128], BF16)
                    pAr = Ring(psp, "pA", 2, [128, 512], F32)
                    pq = Ring(psp, "pq", 1, [128, 512], F32)
                    pkv = Ring(psp, "pkv", 1, [128, 512], F32)
                    pT2 = Ring(psp, "pT2", 2, [128, 8, 128], BF16)
                    v5 = lambda a: a.rearrange("p h (g j e) -> p h g j e", g=2, j=2)

                    def gen1(t):
                        isc = t >= NLT
                        A_, S_, b_af = (AC, SC, b_afc) if isc else (AL, SL, b_afl)
                        xt, b_x = xr.next()
                        k.dma("sp", xt[:], x_src(layer, t), writes=[b_x])
                        if layer == 0:
                            k.dma("act", xres[t * 128:(t + 1) * 128, :], xt[:], reads=[b_x])
                        s4, b_s4 = st4.next()
                        k.op("act", lambda e, xt=xt, s4=s4: e.activation(out=junk[:], in_=xt[:], func=AF.Square,
                                                                         accum_out=s4[:, 0:1]),
                             reads=[b_x], writes=[b_junk, b_s4])
                        rms_rstd(s4[:, 0:1], s4[:, 1:2], D, b_s4, b_s4)
                        h1, b_h1 = h1r.next()
                        hb, b_hb = hbr.next()
                        k.op("dve", lambda e, xt=xt, s4=s4, h1=h1, A_=A_: e.scalar_tensor_tensor(
                            out=h1[:], in0=xt[:], scalar=s4[:, 1:2], in1=A_[:], op0=ALU.mult, op1=ALU.mult),
                            reads=[b_x, b_s4, b_af], writes=[b_h1])
                        k.op("pool", lambda e, h1=h1, hb=hb, S_=S_: e.tensor_tensor(out=hb[:], in0=h1[:], in1=S_[:],
                                                                                    op=ALU.add),
                             reads=[b_h1, b_af], writes=[b_hb])
                        pT, b_pT = pTr.next()
                        for kc in range(8):
                            k.op("pe", lambda e, kc=kc, pT=pT, hb=hb: e.transpose(out=pT[:, kc, :],
                                                                                  in_=hb[:, kc * 128:(kc + 1) * 128],
                                                                                  identity=ident_b[:]),
                                 reads=[b_hb], writes=[b_pT])
                        hT, b_hT = hTr.next()
                        k.op("act", lambda e, hT=hT, pT=pT: e.copy(out=hT[:], in_=pT[:]), reads=[b_pT], writes=[b_hT])
                        k.dma("act", hT_d[t], hT[:].rearrange("p c n -> p (c n)"), reads=[b_hT])
                        pA, b_pA = pAr.next()
                        for kc in range(8):
                            k.op("pe", lambda e, kc=kc, pA=pA, hT=hT: e.matmul(pA[:, 0:352], lhsT=hT[:, kc, :],
                                                                               rhs=winA[:, kc, :], start=(kc == 0),
                                                                               stop=(kc == 7)),
                                 reads=[b_hT, b_w], writes=[b_pA])
                        yield
                        k.op("act", lambda e, pA=pA, s4=s4: e.activation(out=junk[:, 0:192], in_=pA[:, 0:192], func=AF.Square,
                                                                         accum_out=s4[:, 2:3]),
                             reads=[b_pA], writes=[b_junk, b_s4])
                        k.op("act", lambda e, pA=pA, s4=s4: e.activation(out=junk[:, 192:320], in_=pA[:, 192:320],
                                                                         func=AF.Square, accum_out=s4[:, 3:4]),
                             reads=[b_pA], writes=[b_junk, b_s4])
                        rms_rstd(s4[:, 2:3], s4[:, 4:5], 192, b_s4, b_s4)
                        rms_rstd(s4[:, 3:4], s4[:, 5:6], 128, b_s4, b_s4)
                        cn, b_cn = cnr.next()
                        k.op("dve", lambda e, cn=cn, pA=pA, s4=s4: e.tensor_scalar(out=cn[:, 0:192], in0=pA[:, 0:192],
                                                                                   scalar1=s4[:, 4:5], scalar2=None,
                                                                                   op0=ALU.mult),
                             reads=[b_pA, b_s4], writes=[b_cn])
                        k.op("dve", lambda e, cn=cn, pA=pA, s4=s4: e.tensor_scalar(out=cn[:, 192:320], in0=pA[:, 192:320],
                                                                                   scalar1=s4[:, 5:6], scalar2=None,
                                                                                   op0=ALU.mult),
                             reads=[b_pA, b_s4], writes=[b_cn])
                        kpe, b_kpe = kper.next()
                        tmp, b_tmp = tmpr.next()
                        if isc:
                            k.op("act", lambda e, kpe=kpe, pA=pA: e.copy(out=kpe[:], in_=pA[:, 320:352]),
                                 reads=[b_pA], writes=[b_kpe])
                        else:
                            src = v5(pA[:, 320:352].rearrange("p (h n) -> p h n", h=1))
                            dst = v5(kpe[:].rearrange("p (h n) -> p h n", h=1))
                            tc_ = v5(rc[:, t:t + 1, :])
                            ts_ = v5(rs[:, t:t + 1, :])
                            tm = v5(tmp[:, 0:1, :])
                            rope("dve", dst, src, tc_, ts_, tm, [b_pA, b_rt], [b_kpe], b_tmp)
                        p2, b_p2 = pT2.next()
                        k.op("pe", lambda e, p2=p2, cn=cn: e.transpose(out=p2[:, 0, :], in_=cn[:, 0:128], identity=ident_b[:]),
                             reads=[b_cn], writes=[b_p2])
                        k.op("pe", lambda e, p2=p2, cn=cn: e.transpose(out=p2[0:64, 1, :], in_=cn[:, 128:192],
                                                                       identity=ident_b[:]),
                             reads=[b_cn], writes=[b_p2])
                        k.op("pe", lambda e, p2=p2, cn=cn: e.transpose(out=p2[:, 2, :], in_=cn[:, 192:320],
                                                                       identity=ident_b[:]),
                             reads=[b_cn], writes=[b_p2])
                        cTs, b_cT = cTr.next()
                        k.op("act", lambda e, cTs=cTs, p2=p2: e.copy(out=cTs[:, 0, :], in_=p2[:, 0, :]), reads=[b_p2],
                             writes=[b_cT])
                        k.op("act", lambda e, cTs=cTs, p2=p2: e.copy(out=cTs[0:64, 1, :], in_=p2[0:64, 1, :]), reads=[b_p2],
                             writes=[b_cT])
                        k.op("act", lambda e, cTs=cTs, p2=p2: e.copy(out=cTs[:, 2, :], in_=p2[:, 2, :]), reads=[b_p2],
                             writes=[b_cT])
                        need_q = (not isc) or (not last)
                        pq_t, b_pq = pq.next()
                        pkv_t, b_pkv = pkv.next()
                        if need_q:
                            k.op("pe", lambda e, pq_t=pq_t, cTs=cTs: e.matmul(pq_t[:, 0:384], lhsT=cTs[:, 0, :], rhs=wuq0[:],
                                                                              start=True, stop=False),
                                 reads=[b_cT, b_w], writes=[b_pq])
                            k.op("pe", lambda e, pq_t=pq_t, cTs=cTs: e.matmul(pq_t[:, 0:384], lhsT=cTs[0:64, 1, :],
                                                                              rhs=wuq1[:], start=False, stop=True),
                                 reads=[b_cT, b_w], writes=[b_pq])
                        k.op("pe", lambda e, pkv_t=pkv_t, cTs=cTs: e.matmul(pkv_t[:], lhsT=cTs[:, 2, :], rhs=wukv[:],
                                                                            start=True, stop=True),
                             reads=[b_cT, b_w], writes=[b_pkv])
                        yield
                        qf, b_qf = qfr.next()
                        kf, b_kf = kfr.next()
                        pkv3 = pkv_t[:].rearrange("p (h n) -> p h n", h=4)
                        if need_q:
                            qs, b_qs = qsr.next()
                            k.op("act", lambda e, qs=qs, pq_t=pq_t: e.copy(out=qs[:], in_=pq_t[:, 0:384]),
                                 reads=[b_pq], writes=[b_qs])
                            qs3 = qs[:].rearrange("p (h n) -> p h n", h=4)
                            k.op("pool", lambda e, qf=qf, qs3=qs3: e.tensor_copy(out=qf[:, :, 0:64], in_=qs3[:, :, 0:64]),
                                 reads=[b_qs], writes=[b_qf])
                            if isc:
                                k.op("pool", lambda e, qf=qf, qs3=qs3: e.tensor_copy(out=qf[:, :, 64:96], in_=qs3[:, :, 64:96]),
                                     reads=[b_qs], writes=[b_qf])
                            else:
                                src = v5(qs3[:, :, 64:96])
                                dst = v5(qf[:, :, 64:96])
                                tc_ = v5(rc[:, t:t + 1, :].broadcast_to([128, 4, 32]))
                                ts_ = v5(rs[:, t:t + 1, :].broadcast_to([128, 4, 32]))
                                tm = v5(tmp[:])
                                rope("dve", dst, src, tc_, ts_, tm, [b_qs, b_rt], [b_qf], b_tmp)
                        k.op("act", lambda e, kf=kf, pkv3=pkv3: e.copy(out=kf[:, :, 0:64], in_=pkv3[:, :, 0:64]),
                             reads=[b_pkv], writes=[b_kf])
                        k.op("pool", lambda e, kf=kf, kpe=kpe: e.tensor_copy(
                            out=kf[:, :, 64:96], in_=kpe[:].rearrange("p (h n) -> p h n", h=1).broadcast_to([128, 4, 32])),
                            reads=[b_kpe], writes=[b_kf])
                        pkv4 = pkv_t[:].rearrange("p (a j n) -> p a j n", a=2, j=2)
                        k.op("act", lambda e, pkv4=pkv4, t=t: e.copy(out=VA[:, t, :, 0:64], in_=pkv4[:, :, 0, 64:128]),
                             reads=[b_pkv], writes=[b_VA])
                        k.op("act", lambda e, pkv4=pkv4, t=t: e.copy(out=VA[:, t, :, 128:192], in_=pkv4[:, :, 1, 64:128]),
                             reads=[b_pkv], writes=[b_VA])
                        p3, b_p3 = pTr.next()
                        for h in range(4):
                            if need_q:
                                k.op("pe", lambda e, h=h, p3=p3, qf=qf: e.transpose(out=p3[0:96, h, :], in_=qf[:, h, :],
                                                                                    identity=ident_b[:]),
                                     reads=[b_qf], writes=[b_p3])
                            k.op("pe", lambda e, h=h, p3=p3, kf=kf: e.transpose(out=p3[0:96, 4 + h, :], in_=kf[:, h, :],
                                                                                identity=ident_b[:]),
                                 reads=[b_kf], writes=[b_p3])
                        if need_q:
                            k.op("dve", lambda e, p3=p3, t=t: e.tensor_copy(out=QT[:, :, t * 128:(t + 1) * 128],
                                                                            in_=p3[0:96, 0:4, :]), reads=[b_p3], writes=[b_QT])
                        k.op("act", lambda e, p3=p3, t=t: e.copy(out=KT[:, :, t * 128:(t + 1) * 128], in_=p3[0:96, 4:8, :]),
                             reads=[b_p3], writes=[b_KT])
                    run_pipelined(gen1, range(NT), 3)
                    k.barrier()
                if stop_after == ("P1a", layer):
                    break
                with contextlib.ExitStack() as stp:
                    sbp, psp = mk_alloc(stp)
                    pS = Ring(psp, "pS", 4, [128, 512], F32)
                    pO = Ring(psp, "pO", 2, [128, 512], F32)
                    pB = Ring(psp, "pB", 2, [128, 512], F32)
                    PTr = Ring(sbp, "PT", 4, [128, 512], BF16)
                    recr = Ring(sbp, "rec", 2, [128, 512], F32)
                    bcr = Ring(sbp, "bcs", 2, [128, 512], F32)
                    ostg = Ring(sbp, "ostg", 3, [128, 512], BF16)
                    scale = float(96 ** -0.5)
                    qchunks = [(q0, 512, list(range(NT))) for q0 in range(0, L, 512)]
                    if not last:
                        qchunks.append((L, LC, [NLT, NLT + 1]))
                    stg_d = {}

                    def gen_mla(u):
                        if True:
                            q0, N, ktl, h = u
                            pr, odd = h // 2, h % 2
                            if not odd:
                                stg_d[(q0, pr)] = ostg.next()
                            og, b_og = stg_d[(q0, pr)]
                            po, b_po = pO.next()
                            Mlo, Mhi = (64, 192) if odd else (0, 65)
                            pend = []

                            def issue_s(kt, h=h, q0=q0, N=N):
                                pS_t, b_pS = pS.next()
                                k.op("pe", lambda e: e.matmul(pS_t[:, 0:N], lhsT=KT[:, h, kt * 128:(kt + 1) * 128],
                                                              rhs=QT[:, h, q0:q0 + N], start=True, stop=True),
                                     reads=[b_QT, b_KT], writes=[b_pS])
                                pt, b_pt = PTr.next()
                                k.op("act", lambda e: e.activation(out=pt[:, 0:N], in_=pS_t[:, 0:N], func=AF.Exp, scale=scale),
                                     reads=[b_pS], writes=[b_pt])
                                return (kt, pt, b_pt)

                            def issue_o(item, first, lastk, h=h, N=N, po=po, b_po=b_po, pr=pr, Mlo=Mlo, Mhi=Mhi):
                                kt, pt, b_pt = item
                                k.op("pe", lambda e: e.matmul(po[0:Mhi - Mlo, 0:N], lhsT=VA[:, kt, pr, Mlo:Mhi], rhs=pt[:, 0:N],
                                                              start=first, stop=lastk),
                                     reads=[b_pt, b_VA], writes=[b_po])
                            nk = len(ktl)
                            for i, kt in enumerate(ktl):
                                pend.append(issue_s(kt))
                                if len(pend) > 2:
                                    it = pend.pop(0)
                                    issue_o(it, it[0] == ktl[0], False)
                            while pend:
                                it = pend.pop(0)
                                issue_o(it, it[0] == ktl[0], len(pend) == 0)
                            yield
                            pd = 0 if odd else 64
                            pout = 64 if odd else 0
                            rec, b_rec = recr.next()
                            k.op("dve", lambda e, rec=rec, po=po, pd=pd, N=N: e.reciprocal(out=rec[pd:pd + 1, 0:N],
                                                                                           in_=po[pd:pd + 1, 0:N]),
                                 reads=[b_po], writes=[b_rec])
                            pb_t, b_pb = pB.next()
                            k.op("pe", lambda e, pb_t=pb_t, rec=rec, pd=pd, N=N: e.matmul(pb_t[:, 0:N],
                                                                                          lhsT=ones_f[pd:pd + 1, :],
                                                                                          rhs=rec[pd:pd + 1, 0:N],
                                                                                          start=True, stop=True),
                                 reads=[b_rec], writes=[b_pb])
                            bcs, b_bcs = bcr.next()
                            k.op("act", lambda e, bcs=bcs, pb_t=pb_t, pout=pout, N=N: e.copy(out=bcs[pout:pout + 64, 0:N],
                                                                                           in_=pb_t[pout:pout + 64, 0:N]),
                                 reads=[b_pb], writes=[b_bcs])
                            k.op("dve", lambda e, og=og, po=po, bcs=bcs, pout=pout, N=N: e.tensor_tensor(
                                out=og[pout:pout + 64, 0:N], in0=po[pout:pout + 64, 0:N], in1=bcs[pout:pout + 64, 0:N],
                                op=ALU.mult), reads=[b_po, b_bcs], writes=[b_og])
                            if odd:
                                k.dma("sp", mixT_d[pr, :, q0:q0 + N], og[:, 0:N], reads=[b_og])
                    run_pipelined(gen_mla, [(q0, N, ktl, h) for (q0, N, ktl) in qchunks for h in range(4)], 2)
                    k.barrier()
            if stop_after == ("P1", layer):
                break

            with contextlib.ExitStack() as st:
                sb, ps = mk_alloc(st)
                winB = sb("winB", [128, 8, 768], BF16)
                rcB = sb("ropeBc", [128, NLT, 64], F32)
                rsB = sb("ropeBs", [128, NLT, 64], F32)
                mprev = sb("mprev", [128, 512], BF16)
                mnext = sb("mnext", [128, 512], BF16)
                esink = sb("esink", [128, 2, 512], F32)
                QKB = sb("QKB", [128, 8, NT * 128], BF16)
                VB = sb("VB", [128, NT, 2, 192], BF16)
                b_w, b_rt, b_QKB, b_VB, b_es = Buf(), Buf(), Buf(), Buf(), Buf()
                with contextlib.ExitStack() as stw:
                    sbw, _ = mk_alloc(stw)
                    b_s = Buf()
                    for hf in range(2):
                        stg = sbw("stgB%d" % hf, [128, 8, 384], F32)
                        k.dma("sp", stg[:], W["w_in"][layer, :, 352 + hf * 384:352 + (hf + 1) * 384].rearrange(
                            "(c p) n -> p c n", p=128), writes=[b_s])
                        k.op("pool" if hf else "dve", lambda e, stg=stg, hf=hf: e.tensor_copy(
                            out=winB[:, :, hf * 384:(hf + 1) * 384], in_=stg[:]), reads=[b_s], writes=[b_w])
                    mf = sbw("mf", [128, 2, 128], F32)
                    k.dma("sp", mf[:, 0, :], C["mask_prev"][:, :], writes=[b_s])
                    k.dma("sp", mf[:, 1, :], C["mask_next"][:, :], writes=[b_s])
                    k.op("dve", lambda e: e.tensor_copy(out=mprev[:].rearrange("p (r n) -> p r n", r=4),
                                                        in_=mf[:, 0:1, :].broadcast_to([128, 4, 128])), reads=[b_s], writes=[b_w])
                    k.op("dve", lambda e: e.tensor_copy(out=mnext[:].rearrange("p (r n) -> p r n", r=4),
                                                        in_=mf[:, 1:2, :].broadcast_to([128, 4, 128])), reads=[b_s], writes=[b_w])
                    esk = sbw("esk", [128, 8], F32)
                    k.dma("sp", esk[:], W["swa_sink"][layer:layer + 1, :].broadcast_to([128, 8]), writes=[b_s])
                    k.op("act", lambda e: e.activation(out=esk[:], in_=esk[:], func=AF.Exp), reads=[b_s], writes=[b_s])
                    for g in range(2):
                        for half in range(2):
                            for sl in range(2):
                                h = 4 * g + 2 * sl + half
                                o0 = half * 256 + sl * 128
                                k.op("dve", lambda e, g=g, o0=o0, h=h: e.tensor_copy(
                                    out=esink[:, g, o0:o0 + 128], in_=esk[:, h:h + 1].broadcast_to([128, 128])),
                                    reads=[b_s], writes=[b_es])
                    k.dma("sp", rcB[:], C["ropeB_c"][:, :, :], writes=[b_rt])
                    k.dma("sp", rsB[:], C["ropeB_s"][:, :, :], writes=[b_rt])
                    k.op("pool", lambda e: e.memset(VB[:], 0.0), writes=[b_VB])
                    k.op("pool", lambda e: e.memset(VB[:, :, :, 64:65], 1.0), writes=[b_VB])
                    k.barrier()
                with contextlib.ExitStack() as stp:
                    sbp, psp = mk_alloc(stp)
                    hTr = Ring(sbp, "hTb", 3, [128, 8, 128], BF16)
                    qsr = Ring(sbp, "qsb", 3, [128, 512], F32)
                    ksr = Ring(sbp, "ksb", 3, [128, 256], F32)
                    tmpr = Ring(sbp, "rtmpb", 3, [128, 8, 64], F32)
                    qbr = Ring(sbp, "qb", 3, [128, 8, 64], BF16)
                    kdr = Ring(sbp, "kd", 3, [128, 2, 2, 128], BF16)
                    for kt_, kb_ in zip(kdr.tiles, kdr.bufs):
                        k.op("pool", lambda e, kt_=kt_: e.memset(kt_[:], 0.0), writes=[kb_])
                    p1r = Ring(psp, "pB1", 2, [128, 512], F32)
                    p2r = Ring(psp, "pB2", 2, [128, 512], F32)
                    pTr = Ring(psp, "pTb", 2, [128, 8, 128], BF16)
                    v5 = lambda a: a.rearrange("p h (g j e) -> p h g j e", g=2, j=2)
                    def gen2(t):
                        isc = t >= NLT
                        hT, b_hT = hTr.next()
                        k.dma("sp", hT[:].rearrange("p c n -> p (c n)"), hT_d[t], writes=[b_hT])
                        p1, b_p1 = p1r.next()
                        p2, b_p2 = p2r.next()
                        need_q = (not isc) or (not last)
                        for kc in range(8):
                            if need_q:
                                k.op("pe", lambda e, kc=kc, p1=p1, hT=hT: e.matmul(p1[:], lhsT=hT[:, kc, :], rhs=winB[:, kc, 0:512],
                                                                                   start=(kc == 0), stop=(kc == 7)),
                                     reads=[b_hT, b_w], writes=[b_p1])
                        for kc in range(8):
                            k.op("pe", lambda e, kc=kc, p2=p2, hT=hT: e.matmul(p2[:, 0:256], lhsT=hT[:, kc, :],
                                                                               rhs=winB[:, kc, 512:768], start=(kc == 0),
                                                                               stop=(kc == 7)),
                                 reads=[b_hT, b_w], writes=[b_p2])
                        yield
                        qs, b_qs = qsr.next()
                        ksb, b_ks = ksr.next()
                        tmp, b_tmp = tmpr.next()
                        qb, b_qb = qbr.next()
                        kd, b_kd = kdr.next()
                        if need_q:
                            k.op("act", lambda e, qs=qs, p1=p1: e.copy(out=qs[:], in_=p1[:]), reads=[b_p1], writes=[b_qs])
                        k.op("act", lambda e, ksb=ksb, p2=p2: e.copy(out=ksb[:], in_=p2[:, 0:256]), reads=[b_p2], writes=[b_ks])
                        qs3 = qs[:].rearrange("p (h n) -> p h n", h=8)
                        ks3 = ksb[:, 0:128].rearrange("p (h n) -> p h n", h=2)
                        if isc:
                            if need_q:
                                k.op("pool", lambda e, qb=qb, qs3=qs3: e.tensor_copy(out=qb[:], in_=qs3), reads=[b_qs], writes=[b_qb])
                            k.op("pool", lambda e, kd=kd, ks3=ks3: e.tensor_copy(out=kd[:, :, 0, 0:64], in_=ks3), reads=[b_ks],
                                 writes=[b_kd])
                        else:
                            tcq = v5(rcB[:, t:t + 1, :].broadcast_to([128, 8, 64]))
                            tsq = v5(rsB[:, t:t + 1, :].broadcast_to([128, 8, 64]))
                            rope("dve", v5(qb[:]), v5(qs3), tcq, tsq, v5(tmp[:]), [b_qs, b_rt], [b_qb], b_tmp)
                            tck = v5(rcB[:, t:t + 1, :].broadcast_to([128, 2, 64]))
                            tsk = v5(rsB[:, t:t + 1, :].broadcast_to([128, 2, 64]))
                            rope("pool", v5(kd[:, :, 0, 0:64]), v5(ks3), tck, tsk, v5(tmp[:, 0:2, :]), [b_ks, b_rt, b_qb], [b_kd],
                                 b_tmp)
                        k.op("pool", lambda e, kd=kd: e.tensor_copy(out=kd[:, :, 1, 64:128], in_=kd[:, :, 0, 0:64]), reads=[b_kd],
                             writes=[b_kd])
                        sv3 = ksb[:, 128:256].rearrange("p (g n) -> p g n", g=2)
                        k.op("pool", lambda e, sv3=sv3, t=t: e.tensor_copy(out=VB[:, t, :, 0:64], in_=sv3), reads=[b_ks],
                             writes=[b_VB])
                        k.op("pool", lambda e, sv3=sv3, t=t: e.tensor_copy(out=VB[:, t, :, 128:192], in_=sv3), reads=[b_ks],
                             writes=[b_VB])
                        yield
                        pT, b_pT = pTr.next()
                        if need_q:
                            for p in range(4):
                                k.op("pe", lambda e, p=p, pT=pT, qb=qb: e.transpose(
                                    out=pT[:, p, :], in_=qb[:, 2 * p:2 * p + 2, :].rearrange("p h n -> p (h n)"),
                                    identity=ident_b[:]), reads=[b_qb], writes=[b_pT])
                        for g in range(2):
                            for v_ in range(2):
                                k.op("pe", lambda e, g=g, v_=v_, pT=pT, kd=kd: e.transpose(
                                    out=pT[:, 4 + 2 * g + v_, :], in_=kd[:, g, v_, :], identity=ident_b[:]),
                                    reads=[b_kd], writes=[b_pT])
                        if need_q:
                            k.op("dve", lambda e, pT=pT, t=t: e.tensor_copy(out=QKB[:, :, t * 128:(t + 1) * 128], in_=pT[:, 0:8, :]),
                                 reads=[b_pT], writes=[b_QKB])
                        else:
                            k.op("dve", lambda e, pT=pT, t=t: e.tensor_copy(out=QKB[:, 4:8, t * 128:(t + 1) * 128],
                                                                            in_=pT[:, 4:8, :]), reads=[b_pT], writes=[b_QKB])
                    run_pipelined(gen2, range(NT), 3)
                    k.barrier()
                if stop_after == ("P2a", layer):
                    break
                with contextlib.ExitStack() as stp:
                    sbp, psp = mk_alloc(stp)
                    pS = Ring(psp, "pSb", 4, [128, 512], F32)
                    pO = Ring(psp, "pOb", 2, [128, 512], F32)
                    pB = Ring(psp, "pBb", 2, [128, 512], F32)
                    PTr = Ring(sbp, "PTb", 4, [128, 512], BF16)
                    recr = Ring(sbp, "recb", 2, [128, 512], F32)
                    bcr = Ring(sbp, "bcsb", 2, [128, 512], F32)
                    stg_r = [Ring(sbp, "ostb%d" % g, 2, [128, 4, 512], BF16) for g in range(2)]
                    blocks = list(range(NLT)) + ([] if last else [NLT, NLT + 1])
                    cur = [None, None]
                    def gen_swa(u):
                        i, g = u
                        isc = i >= NLT
                        if isc:
                            ktl = [(NLT, None), (NLT + 1, None)]
                            base, off, span = NLT, (i - NLT) * 128, 256
                        else:
                            ktl = ([(i - 1, mprev)] if i > 0 else []) + [(i, None)] + ([(i + 1, mnext)] if i < NLT - 1 else []) \
                                + [(NLT, None), (NLT + 1, None)]
                            base, off, span = (i // 4) * 4, (i % 4) * 128, 512
                        if True:
                            if off == 0:
                                cur[g] = stg_r[g].next()
                            og, b_og = cur[g]
                            po, b_po = pO.next()
                            pend = []

                            def pv(item, lastk, po=po, b_po=b_po, g=g):
                                kt2, pt2, b_pt2, n2 = item
                                k.op("pe", lambda e: e.matmul(po[0:65, :], lhsT=VB[:, kt2, g, 0:65], rhs=pt2[:], start=(n2 == 0),
                                                              stop=lastk), reads=[b_pt2, b_VB], writes=[b_po])
                            for n_, (kt, msk) in enumerate(ktl):
                                pS_t, b_pS = pS.next()
                                if msk is not None:
                                    k.op("pe", lambda e, pS_t=pS_t, msk=msk: e.matmul(pS_t[:], lhsT=ident_b[:], rhs=msk[:], start=True,
                                                                                      stop=False, skip_group_check=True),
                                         reads=[b_w], writes=[b_pS])
                                for half in range(2):
                                    k.op("pe", lambda e, pS_t=pS_t, half=half, kt=kt, g=g, i=i, msk=msk: e.matmul(
                                        pS_t[:, half * 256:(half + 1) * 256],
                                        lhsT=QKB[:, 4 + 2 * g + half, kt * 128:(kt + 1) * 128],
                                        rhs=QKB[:, 2 * g:2 * g + 2, i * 128:(i + 1) * 128],
                                        start=(msk is None), stop=True, skip_group_check=True),
                                        reads=[b_QKB], writes=[b_pS])
                                pt, b_pt = PTr.next()
                                k.op("act", lambda e, pt=pt, pS_t=pS_t: e.activation(out=pt[:], in_=pS_t[:], func=AF.Exp, scale=0.125),
                                     reads=[b_pS], writes=[b_pt])
                                pend.append((kt, pt, b_pt, n_))
                                if len(pend) > 1:
                                    pv(pend.pop(0), False)
                                yield
                            pv(pend.pop(0), True)
                            yield
                            rec, b_rec = recr.next()
                            k.op("dve", lambda e, rec=rec, po=po, g=g: e.tensor_tensor(
                                out=rec[64:65, :], in0=po[64:65, :], in1=esink[64:65, g, :], op=ALU.add),
                                reads=[b_po, b_es], writes=[b_rec])
                            k.op("dve", lambda e, rec=rec: e.reciprocal(out=rec[64:65, :], in_=rec[64:65, :]),
                                 reads=[b_rec], writes=[b_rec])
                            pb_t, b_pb = pB.next()
                            k.op("pe", lambda e, pb_t=pb_t, rec=rec: e.matmul(pb_t[:], lhsT=ones_f[64:65, :], rhs=rec[64:65, :],
                                                                              start=True, stop=True), reads=[b_rec], writes=[b_pb])
                            bcs, b_bcs = bcr.next()
                            k.op("act", lambda e, bcs=bcs, pb_t=pb_t: e.copy(out=bcs[0:64, :], in_=pb_t[0:64, :]), reads=[b_pb],
                                 writes=[b_bcs])
                            k.op("dve", lambda e, og=og, po=po, bcs=bcs, off=off: e.tensor_tensor(
                                out=og[0:64, :, off:off + 128], in0=po[0:64, :].rearrange("p (s n) -> p s n", s=4),
                                in1=bcs[0:64, :].rearrange("p (s n) -> p s n", s=4), op=ALU.mult),
                                reads=[b_po, b_bcs], writes=[b_og])
                            if off + 128 == span:
                                for half in range(2):
                                    k.dma("sp", mixT_d[2 + 2 * g:4 + 2 * g, half * 64:(half + 1) * 64,
                                                       base * 128:base * 128 + span].rearrange("c p n -> p c n"),
                                          og[0:64, half * 2:(half + 1) * 2, 0:span], reads=[b_og])
                    run_pipelined(gen_swa, [(i, g) for i in blocks for g in range(2)], 2)
                    k.barrier()
            if stop_after == ("P2", layer):
                break

            with contextlib.ExitStack() as st:
                sb, ps = mk_alloc(st)
                Gs = sb("Gs", [128, NT, 256], BF16)
                Vc = sb("Vc", [128, NT, 256], BF16)
                Kd = sb("Kd", [128, NT, 2, 256], BF16)
                QKc = sb("QKc", [128, 6, NT * 128], BF16)
                Mk = sb("Mk", [128, 4, 128], F32)
                qdec = sb("qdec", [128, 8], F32)
                kdec = sb("kdec", [128, 8], F32)
                cdT = sb("cdT", [128, 2, 2, 64], F32)
                b_Gs, b_Vc, b_Kd, b_QKc, b_cst = Buf(), Buf(), Buf(), Buf(), Buf()
                with contextlib.ExitStack() as stw:
                    sbw, _ = mk_alloc(stw)
                    b_s = Buf()
                    lgR = sbw("lgR", [128, 8], F32)
                    lgP = sbw("lgP", [128, 4], F32)
                    for d_, nm in enumerate(("ret_decay_fwd", "ret_decay_bwd")):
                        k.dma("sp", lgR[:, d_ * 4:(d_ + 1) * 4], W[nm][layer:layer + 1, :].broadcast_to([128, 4]), writes=[b_s])
                        for two in range(2):
                            for pr in range(2):
                                hh = 2 * pr + two
                                k.dma("sp", lgP[two * 64:(two + 1) * 64, d_ * 2 + pr:d_ * 2 + pr + 1],
                                      W[nm][layer:layer + 1, hh:hh + 1].broadcast_to([64, 1]), writes=[b_s])
                    for tl in (lgR, lgP):
                        k.op("act", lambda e, tl=tl: e.activation(out=tl[:], in_=tl[:], func=AF.Exp, scale=-1.0), reads=[b_s],
                             writes=[b_s])
                        k.op("dve", lambda e, tl=tl: e.tensor_scalar(out=tl[:], in0=tl[:], scalar1=1.0, scalar2=None, op0=ALU.add),
                             reads=[b_s], writes=[b_s])
                        k.op("act", lambda e, tl=tl: e.activation(out=tl[:], in_=tl[:], func=AF.Ln), reads=[b_s], writes=[b_s])
                        k.op("dve", lambda e, tl=tl: e.tensor_scalar(out=tl[:], in0=tl[:], scalar1=-1.0, scalar2=None, op0=ALU.mult),
                             reads=[b_s], writes=[b_s])
                    cf = sbw("retc", [128, 6, 128], F32)
                    for i_, nm in enumerate(("ret_d1", "ret_i1", "ret_d2", "ret_i2", "ret_j1", "ret_j2")):
                        k.dma("sp", cf[:, i_, :], C[nm][:, :], writes=[b_s])
                    pc = sbw("retpc", [128, 4], F32)
                    k.dma("sp", pc[:], C["ret_pc"][:, :], writes=[b_s])
                    e1 = sbw("e1", [128, 128], F32)
                    e2 = sbw("e2", [128, 128], F32)
                    for h in range(4):
                        k.op("act", lambda e, h=h: e.activation(out=e1[:], in_=cf[:, 0, :], func=AF.Exp, scale=lgR[:, h:h + 1]),
                             reads=[b_s], writes=[b_s])
                        k.op("dve", lambda e: e.tensor_tensor(out=e1[:], in0=e1[:], in1=cf[:, 1, :], op=ALU.mult), reads=[b_s],
                             writes=[b_s])
                        k.op("act", lambda e, h=h: e.activation(out=e2[:], in_=cf[:, 2, :], func=AF.Exp, scale=lgR[:, 4 + h:5 + h]),
                             reads=[b_s], writes=[b_s])
                        k.op("dve", lambda e: e.tensor_tensor(out=e2[:], in0=e2[:], in1=cf[:, 3, :], op=ALU.mult), reads=[b_s],
                             writes=[b_s])
                        k.op("dve", lambda e, h=h: e.tensor_tensor(out=Mk[:, h, :], in0=e1[:], in1=e2[:], op=ALU.add), reads=[b_s],
                             writes=[b_cst])
                    k.op("dve", lambda e: e.tensor_scalar(out=qdec[:, 0:4], in0=lgR[:, 0:4], scalar1=pc[:, 2:3], scalar2=None,
                                                          op0=ALU.mult), reads=[b_s], writes=[b_cst])
                    k.op("dve", lambda e: e.tensor_scalar(out=qdec[:, 4:8], in0=lgR[:, 4:8], scalar1=pc[:, 3:4], scalar2=None,
                                                          op0=ALU.mult), reads=[b_s], writes=[b_cst])
                    k.op("act", lambda e: e.activation(out=qdec[:], in_=qdec[:], func=AF.Exp), reads=[b_cst], writes=[b_cst])
                    k.op("dve", lambda e: e.tensor_scalar(out=kdec[:, 0:4], in0=lgR[:, 0:4], scalar1=pc[:, 0:1], scalar2=None,
                                                          op0=ALU.mult), reads=[b_s], writes=[b_cst])
                    k.op("dve", lambda e: e.tensor_scalar(out=kdec[:, 4:8], in0=lgR[:, 4:8], scalar1=pc[:, 1:2], scalar2=None,
                                                          op0=ALU.mult), reads=[b_s], writes=[b_cst])
                    k.op("act", lambda e: e.activation(out=kdec[:], in_=kdec[:], func=AF.Exp), reads=[b_cst], writes=[b_cst])
                    k.op("act", lambda e: e.activation(out=lgP[:], in_=lgP[:], func=AF.Exp, scale=128.0), reads=[b_s], writes=[b_s])
                    k.op("dve", lambda e: e.tensor_copy(out=cdT[:].rearrange("p a b n -> p (a b) n"),
                                                        in_=lgP[:, 0:4].rearrange("p (c o) -> p c o", o=1).broadcast_to([128, 4, 64])),
                         reads=[b_s], writes=[b_cst])
                    k.barrier()
                with contextlib.ExitStack() as stp:
                    sbp, psp = mk_alloc(stp)
                    winC = sbp("winC", [128, 8, 1024], BF16)
                    b_w = Buf()
                    with contextlib.ExitStack() as stw:
                        sbw, _ = mk_alloc(stw)
                        b_s = Buf()
                        for hf in range(2):
                            stg = sbw("stgC%d" % hf, [128, 8, 512], F32)
                            k.dma("sp", stg[:], W["w_in"][layer, :, 1120 + hf * 512:1120 + (hf + 1) * 512].rearrange(
                                "(c p) n -> p c n", p=128), writes=[b_s])
                            k.op("pool" if hf else "dve", lambda e, stg=stg, hf=hf: e.tensor_copy(
                                out=winC[:, :, hf * 512:(hf + 1) * 512], in_=stg[:]), reads=[b_s], writes=[b_w])
                        k.barrier()
                    hTr = Ring(sbp, "hTc", 3, [128, 8, 128], BF16)
                    rtc = Ring(sbp, "rtc", 3, [128, 2, 2, 64], F32)
                    qkr = Ring(sbp, "qkc", 3, [128, 512], F32)
                    tmpr = Ring(sbp, "rtmpc", 4, [128, 4, 64], F32)
                    qcr = Ring(sbp, "qc", 3, [128, 2, 4, 64], BF16)
                    kzr = Ring(sbp, "kz", 3, [128, 2, 2, 128], BF16)
                    for kt_, kb_ in zip(kzr.tiles, kzr.bufs):
                        k.op("pool", lambda e, kt_=kt_: e.memset(kt_[:], 0.0), writes=[kb_])
                    p1r = Ring(psp, "pC1", 2, [128, 512], F32)
                    p2r = Ring(psp, "pC2", 2, [128, 512], F32)
                    pTr = Ring(psp, "pTc", 2, [128, 8, 128], BF16)
                    v5c = lambda a: a.rearrange("p h (g j e) -> p h g j e", g=1, j=2)
                    def gen3(t):
                        isc = t >= NLT
                        hT, b_hT = hTr.next()
                        k.dma("sp", hT[:].rearrange("p c n -> p (c n)"), hT_d[t], writes=[b_hT])
                        p1, b_p1 = p1r.next()
                        p2, b_p2 = p2r.next()
                        for kc in range(8):
                            k.op("pe", lambda e, kc=kc, p1=p1, hT=hT: e.matmul(p1[:], lhsT=hT[:, kc, :], rhs=winC[:, kc, 0:512],
                                                                               start=(kc == 0), stop=(kc == 7)),
                                 reads=[b_hT, b_w], writes=[b_p1])
                        for kc in range(8):
                            k.op("pe", lambda e, kc=kc, p2=p2, hT=hT: e.matmul(p2[:], lhsT=hT[:, kc, :], rhs=winC[:, kc, 512:1024],
                                                                               start=(kc == 0), stop=(kc == 7)),
                                 reads=[b_hT, b_w], writes=[b_p2])
                        yield
                        qk, b_qk = qkr.next()
                        k.op("act", lambda e, qk=qk, p1=p1: e.copy(out=qk[:], in_=p1[:]), reads=[b_p1], writes=[b_qk])
                        k.op("act", lambda e, p2=p2, t=t: e.activation(out=Gs[:, t, :], in_=p2[:, 256:512], func=AF.Silu),
                             reads=[b_p2], writes=[b_Gs])
                        k.op("act", lambda e, p2=p2, t=t: e.copy(out=Vc[:, t, :], in_=p2[:, 0:256]), reads=[b_p2], writes=[b_Vc])
                        qc, b_qc = qcr.next()
                        tmp, b_tmp = tmpr.next()
                        qk4 = qk[:].rearrange("p (a h n) -> p a h n", a=2, h=4)
                        if isc:
                            k.op("pool", lambda e, qc=qc, qk4=qk4: e.tensor_copy(out=qc[:, 0, :, :], in_=qk4[:, 0, :, :]),
                                 reads=[b_qk], writes=[b_qc])
                            k.op("pool", lambda e, qc=qc, qk4=qk4: e.tensor_scalar(out=qc[:, 1, :, :], in0=qk4[:, 1, :, :],
                                                                                   scalar1=0.125, scalar2=None, op0=ALU.mult),
                                 reads=[b_qk], writes=[b_qc])
                        else:
                            rt, b_rt = rtc.next()
                            k.dma("sp", rt[:, 0, :, :], C["ropeC_c"][:, t, :, :], writes=[b_rt])
                            k.dma("sp", rt[:, 1, :, :], C["ropeC_s"][:, t, :, :], writes=[b_rt])
                            for a_, eng in ((0, "dve"), (1, "pool")):
                                tc_ = v5c(rt[:, 0, a_:a_ + 1, :].broadcast_to([128, 4, 64]))
                                ts_ = v5c(rt[:, 1, a_:a_ + 1, :].broadcast_to([128, 4, 64]))
                                b_t2 = Buf()
                                tm = tmp if a_ == 0 else tmpr.next()[0]
                                rope(eng, v5c(qc[:, a_, :, :]), v5c(qk4[:, a_, :, :]), tc_, ts_, v5c(tm[:]), [b_qk, b_rt], [b_qc],
                                     b_tmp if a_ == 0 else tmpr.bufs[(tmpr.i - 1) % 4])
                        for d_ in range(2):
                            k.op("pool" if d_ else "dve", lambda e, qc=qc, d_=d_, t=t: e.tensor_tensor(
                                out=Kd[:, t, d_, :].rearrange("p (h n) -> p h n", h=4), in0=qc[:, 1, :, :],
                                in1=kdec[:, d_ * 4:(d_ + 1) * 4].rearrange("p (h o) -> p h o", o=1).broadcast_to([128, 4, 64]),
                                op=ALU.mult), reads=[b_qc, b_cst], writes=[b_Kd])
                        yield
                        kz, b_kz = kzr.next()
                        qc5 = qc[:, 1, :, :].rearrange("p (pr hl) n -> p pr hl n", hl=2)
                        for hl in range(2):
                            k.op("pool", lambda e, kz=kz, qc5=qc5, hl=hl: e.tensor_copy(
                                out=kz[:, :, hl, hl * 64:(hl + 1) * 64], in_=qc5[:, :, hl, :]), reads=[b_qc], writes=[b_kz])
                        pT, b_pT = pTr.next()
                        for pr in range(2):
                            k.op("pe", lambda e, pr=pr, pT=pT, qc=qc: e.transpose(
                                out=pT[:, pr, :], in_=qc[:, 0, 2 * pr:2 * pr + 2, :].rearrange("p h n -> p (h n)"),
                                identity=ident_b[:]), reads=[b_qc], writes=[b_pT])
                            for hl in range(2):
                                k.op("pe", lambda e, pr=pr, hl=hl, pT=pT, kz=kz: e.transpose(
                                    out=pT[:, 2 + 2 * pr + hl, :], in_=kz[:, pr, hl, :], identity=ident_b[:]),
                                    reads=[b_kz], writes=[b_pT])
                        k.op("act", lambda e, pT=pT, t=t: e.copy(out=QKc[:, :, t * 128:(t + 1) * 128], in_=pT[:, 0:6, :]),
                             reads=[b_pT], writes=[b_QKc])
                    run_pipelined(gen3, range(NT), 3)
                    k.barrier()
                if stop_after == ("P3a", layer):
                    break
                with contextlib.ExitStack() as stp:
                    sbp, psp = mk_alloc(stp)
                    Sst = sbp("Sst", [128, 2, 2, NT, 128], BF16)
                    b_Sst = Buf()
                    k.op("pool", lambda e: e.memset(Sst[:], 0.0), writes=[b_Sst])
                    pUr = Ring(psp, "pU", 1, [128, 512], F32)
                    for d_ in range(2):
                        order = [NLT, NLT + 1] + list(range(NLT)) if d_ == 0 else [NLT + 1, NLT] + list(range(NLT - 1, -1, -1))
                        S = sbp("S%d" % d_, [128, 2, 64], F32)
                        b_S = Buf()
                        k.op("pool", lambda e, S=S: e.memset(S[:], 0.0), writes=[b_S])
                        for t in order:
                            for hl in range(2):
                                k.op("act", lambda e, S=S, d_=d_, t=t, hl=hl: e.copy(
                                    out=Sst[hl * 64:(hl + 1) * 64, d_, :, t, hl * 64:(hl + 1) * 64], in_=S[hl * 64:(hl + 1) * 64, :, :]),
                                    reads=[b_S], writes=[b_Sst])
                            if t == order[-1]:
                                break
                            pU, b_pU = pUr.next()
                            for pr in range(2):
                                k.op("pe", lambda e, pU=pU, pr=pr, t=t, d_=d_: e.matmul(
                                    pU[:, pr * 128:(pr + 1) * 128], lhsT=Kd[:, t, d_, pr * 128:(pr + 1) * 128],
                                    rhs=Vc[:, t, pr * 128:(pr + 1) * 128], start=True, stop=True, skip_group_check=True),
                                    reads=[b_Kd, b_Vc], writes=[b_pU])
                            k.op("dve", lambda e, S=S, d_=d_: e.tensor_tensor(out=S[:], in0=S[:], in1=cdT[:, d_, :, :], op=ALU.mult),
                                 reads=[b_S, b_cst], writes=[b_S])
                            for hl in range(2):
                                k.op("dve", lambda e, S=S, pU=pU, hl=hl: e.tensor_tensor(
                                    out=S[hl * 64:(hl + 1) * 64, :, :], in0=S[hl * 64:(hl + 1) * 64, :, :],
                                    in1=pU[hl * 64:(hl + 1) * 64, 0:256].rearrange("p (a n) -> p a n", a=2)[:, :, hl * 64:(hl + 1) * 64],
                                    op=ALU.add), reads=[b_S, b_pU], writes=[b_S])
                    pAr = Ring(psp, "pAc", 2, [128, 512], F32)
                    pOr = Ring(psp, "pOc", 2, [128, 512], F32)
                    pQr = Ring(psp, "pQc", 2, [128, 512], F32)
                    oar = Ring(sbp, "oa", 2, [128, 512], F32)
                    pTr = Ring(psp, "pTo", 1, [128, 8, 128], BF16)
                    ATr = Ring(sbp, "AT", 2, [128, 512], BF16)
                    s4r = Ring(sbp, "s4c", 2, [128, 8], F32)
                    junk = sbp("junkc", [128, 64], F32)
                    b_junk = Buf()
                    o1r = Ring(sbp, "o1", 2, [128, 256], F32)
                    o2r = Ring(sbp, "o2", 2, [128, 256], BF16)
                    ostg = Ring(sbp, "ostc", 2, [128, 2, 512], BF16)
                    curd = {}

                    def gen_ro(t):
                        isc = t >= NLT
                        base, off, span = (NLT, (t - NLT) * 128, 256) if isc else ((t // 4) * 4, (t % 4) * 128, 512)
                        if off == 0:
                            curd[base] = ostg.next()
                        og, b_og = curd[base]
                        pA, b_pA = pAr.next()
                        for h in range(4):
                            pr, hl = h // 2, h % 2
                            k.op("pe", lambda e, pA=pA, h=h, pr=pr, hl=hl, t=t: e.matmul(
                                pA[:, h * 128:(h + 1) * 128], lhsT=QKc[:, 2 + 2 * pr + hl, t * 128:(t + 1) * 128],
                                rhs=QKc[:, pr, t * 128:(t + 1) * 128], start=True, stop=True,
                                skip_group_check=True), reads=[b_QKc], writes=[b_pA])
                        AT, b_AT = ATr.next()
                        k.op("dve", lambda e, AT=AT, pA=pA: e.tensor_tensor(out=AT[:], in0=pA[:],
                                                                            in1=Mk[:].rearrange("p h n -> p (h n)"), op=ALU.mult),
                             reads=[b_pA, b_cst], writes=[b_AT])
                        pO, b_pO = pOr.next()
                        pQ, b_pQ = pQr.next()
                        for h in range(4):
                            k.op("pe", lambda e, pO=pO, AT=AT, h=h, t=t: e.matmul(
                                pO[:, h * 64:(h + 1) * 64], lhsT=AT[:, h * 128:(h + 1) * 128], rhs=Vc[:, t, h * 64:(h + 1) * 64],
                                start=True, stop=True, skip_group_check=True), reads=[b_AT, b_Vc], writes=[b_pO])
                        for d_ in range(2):
                            for pr in range(2):
                                k.op("pe", lambda e, pQ=pQ, t=t, d_=d_, pr=pr: e.matmul(
                                    pQ[:, d_ * 256 + pr * 128:d_ * 256 + (pr + 1) * 128], lhsT=QKc[:, pr, t * 128:(t + 1) * 128],
                                    rhs=Sst[:, d_, pr, t, :], start=True, stop=True, skip_group_check=True),
                                    reads=[b_QKc, b_Sst], writes=[b_pQ])
                        yield
                        oa, b_oa = oar.next()
                        k.op("dve", lambda e, oa=oa, pQ=pQ: e.tensor_tensor(
                            out=oa[:].rearrange("p (a h n) -> p a h n", a=2, h=4),
                            in0=pQ[:].rearrange("p (a h n) -> p a h n", a=2, h=4),
                            in1=qdec[:].rearrange("p (a h o) -> p a h o", a=2, o=1).broadcast_to([128, 2, 4, 64]), op=ALU.mult),
                            reads=[b_pQ, b_cst], writes=[b_oa])
                        k.op("pool", lambda e, oa=oa: e.tensor_tensor(out=oa[:, 0:256], in0=oa[:, 0:256], in1=oa[:, 256:512], op=ALU.add),
                             reads=[b_oa], writes=[b_oa])
                        k.op("dve", lambda e, oa=oa, pO=pO: e.tensor_tensor(out=oa[:, 0:256], in0=pO[:, 0:256], in1=oa[:, 0:256], op=ALU.add),
                             reads=[b_pO, b_oa], writes=[b_oa])
                        pO, b_pO = oa, b_oa
                        s4, b_s4 = s4r.next()
                        for h in range(4):
                            k.op("act", lambda e, pO=pO, s4=s4, h=h: e.activation(out=junk[:], in_=pO[:, h * 64:(h + 1) * 64],
                                                                                 func=AF.Square, accum_out=s4[:, h:h + 1]),
                                 reads=[b_pO], writes=[b_junk, b_s4])
                        rms_rstd(s4[:, 0:4], s4[:, 4:8], 64, b_s4, b_s4)
                        o1, b_o1 = o1r.next()
                        o2, b_o2 = o2r.next()
                        k.op("dve", lambda e, o1=o1, pO=pO, s4=s4: e.tensor_tensor(
                            out=o1[:].rearrange("p (h n) -> p h n", h=4), in0=pO[:, 0:256].rearrange("p (h n) -> p h n", h=4),
                            in1=s4[:, 4:8].rearrange("p (h o) -> p h o", o=1).broadcast_to([128, 4, 64]), op=ALU.mult),
                            reads=[b_pO, b_s4], writes=[b_o1])
                        k.op("pool", lambda e, o1=o1, o2=o2, t=t: e.tensor_tensor(out=o2[:], in0=o1[:], in1=Gs[:, t, :], op=ALU.mult),
                             reads=[b_o1, b_Gs], writes=[b_o2])
                        pT, b_pT = pTr.next()
                        for pr in range(2):
                            k.op("pe", lambda e, pT=pT, o2=o2, pr=pr: e.transpose(out=pT[:, pr, :], in_=o2[:, pr * 128:(pr + 1) * 128],
                                                                                 identity=ident_b[:]), reads=[b_o2], writes=[b_pT])
                        k.op("act", lambda e, og=og, pT=pT, off=off: e.copy(out=og[:, :, off:off + 128], in_=pT[:, 0:2, :]),
                             reads=[b_pT], writes=[b_og])
                        if off + 128 == span:
                            k.dma("sp", mixT_d[6:8, :, base * 128:base * 128 + span].rearrange("c p n -> p c n"), og[:, :, 0:span],
                                  reads=[b_og])
                    run_pipelined(gen_ro, range(ntile), 2)
                    k.barrier()
            if stop_after == ("P3", layer):
                break

            b_xr = [Buf() for _ in range(NT)]
            with contextlib.ExitStack() as st:
                sb, ps = mk_alloc(st)
                wo = [sb("woL", [128, 8, D], BF16), None if last else sb("woC", [128, 8, D], BF16)]
                b_w = Buf()
                with contextlib.ExitStack() as stw:
                    sbw, _ = mk_alloc(stw)
                    b_s = Buf()
                    g1 = [sbw("g1L", [128, D], F32), sbw("g1C", [128, D], F32)]
                    for wh in range(1 if last else 2):
                        load_rep(g1[wh][:], mod_row(wh, 2), b_s)
                    for hf in range(2):
                        stg = sbw("stgO%d" % hf, [128, 8, 512], F32)
                        k.dma("sp", stg[:], W["w_out"][layer, :, hf * 512:(hf + 1) * 512].rearrange("(c p) n -> p c n", p=128),
                              writes=[b_s])
                        for wh in range(1 if last else 2):
                            k.op("pool" if wh else "dve", lambda e, stg=stg, hf=hf, wh=wh: e.tensor_tensor(
                                out=wo[wh][:, :, hf * 512:(hf + 1) * 512], in0=stg[:],
                                in1=g1[wh][:, hf * 512:(hf + 1) * 512].rearrange("p (o n) -> p o n", o=1).broadcast_to([128, 8, 512]),
                                op=ALU.mult), reads=[b_s], writes=[b_w])
                    k.barrier()
                mTr = Ring(sb, "mT", 2, [128, 8, 512], BF16)
                xr = Ring(sb, "xt4", 3, [128, D], F32)
                pYr = Ring(ps, "pY4", 4, [128, 512], F32)
                for t0 in range(0, ntile, 4):
                    nt_ = min(4, ntile - t0)
                    mT, b_mT = mTr.next()
                    k.dma("sp", mT[:, :, 0:nt_ * 128], mixT_d[:, :, t0 * 128:(t0 + nt_) * 128].rearrange("c p n -> p c n"),
                          writes=[b_mT])
                    for j in range(nt_):
                        t = t0 + j
                        wsel = wo[1] if t >= NLT else wo[0]
                        xt, b_x = xr.next()
                        k.dma("sp", xt[:], xres[t * 128:(t + 1) * 128, :], reads=[b_xr[t]], writes=[b_x])
                        for hf in range(2):
                            pY, b_pY = pYr.next()
                            for c in range(8):
                                k.op("pe", lambda e, pY=pY, mT=mT, c=c, j=j, hf=hf, wsel=wsel: e.matmul(
                                    pY[:], lhsT=mT[:, c, j * 128:(j + 1) * 128], rhs=wsel[:, c, hf * 512:(hf + 1) * 512],
                                    start=(c == 0), stop=(c == 7)), reads=[b_mT, b_w], writes=[b_pY])
                            k.op("dve", lambda e, xt=xt, pY=pY, hf=hf: e.tensor_tensor(
                                out=xt[:, hf * 512:(hf + 1) * 512], in0=pY[:], in1=xt[:, hf * 512:(hf + 1) * 512], op=ALU.add),
                                reads=[b_pY, b_x], writes=[b_x])
                        k.dma("act", xres[t * 128:(t + 1) * 128, :], xt[:], reads=[b_x], writes=[b_xr[t]])
                k.barrier()
            if stop_after == ("P4", layer):
                break

            moe = (layer % 2 == 1)
            with contextlib.ExitStack() as st:
                sb, ps = mk_alloc(st)
                if moe:
                    nexp, nch = NE, D_FFE // 128
                    wsrc = lambda e_: (W["moe_w_gate"][0, e_], W["moe_w_up"][0, e_], W["moe_w_down"][0, e_])
                else:
                    nexp, nch = 1, D_FF // 128
                    wsrc = lambda e_: (W["ffn_w_gate"][0], W["ffn_w_up"][0], W["ffn_w_down"][0])
                FB = 4
                fblocks = [(c0, min(FB, nch - c0)) for c0 in range(0, nch, FB)]
                tblocks = [(t0, 8, 0) for t0 in range(0, NLT, 8)] + ([] if last else [(NLT, 2, 1)])
                acc = sb("acc", [128, 8, D], F32)
                h2T = sb("h2T", [128, 8, 1024], BF16)
                gates = sb("gates", [128, 8, 8], F32)
                b_acc = [Buf() for _ in range(8)]
                b_h2T, b_gates = Buf(), Buf()
                wgr = Ring(sb, "wg", 2, [128, 8, FB * 128], BF16)
                wur = Ring(sb, "wu", 2, [128, 8, FB * 128], BF16)
                wdr = Ring(sb, "wd", 2, [128, FB, D], BF16)
                stgr = Ring(sb, "stgF", 3, [128, 4096], F32)
                actr = Ring(sb, "actT", 2, [128, FB, 512], BF16)
                sgr = Ring(sb, "sg", 2, [128, 512], BF16)
                h2r = Ring(sb, "h2f", 3, [128, D], F32)
                hbr = Ring(sb, "h2b", 3, [128, D], BF16)
                s4r = Ring(sb, "s4f", 4, [128, 8], F32)
                junk = sb("junkf", [128, D], BF16)
                b_junk = Buf()
                psG = Ring(ps, "psG", 2, [128, 512], F32)
                psU = Ring(ps, "psU", 2, [128, 512], F32)
                psY = Ring(ps, "psY", 2, [128, 512], F32)
                psT = Ring(ps, "psT", 1, [128, 8, 128], BF16)
                psR = Ring(ps, "psR", 1, [128, 4, 128], F32)
                if moe:
                    rtf = sb("rtf", [128, 8, 8], F32)
                    h2Tf = sb("h2Tf", [128, 8, 128], F32)
                    b_rtf, b_h2Tf = Buf(), Buf()
                    k.dma("sp", rtf[:], W["moe_router"][0].rearrange("(c p) n -> p c n", p=128), writes=[b_rtf])
                    g8 = Ring(sb, "g8", 3, [128, 32], F32)
                if last:
                    gF = sb("gF", [128, D], F32)
                    b_gF = Buf()
                    load_rep(gF[:], W["final_norm_g"].rearrange("(o n) -> o n", o=1), b_gF)
                    outr = Ring(sb, "outt", 2, [128, D], F32)
                aff = {}
                for wh in sorted(set(tb[2] for tb in tblocks)):
                    A2, S2, b_af = load_affine(sb, "n2%d" % wh, wh, W["norm2_g"], 4, 3)
                    G2 = sb("g2r%d" % wh, [128, D], F32)
                    load_rep(G2[:], mod_row(wh, 5), b_af)
                    aff[wh] = (A2, S2, G2, b_af)
                k.barrier()
                for (t0, ntb, wh) in tblocks:
                    A2, S2, G2, b_af = aff[wh]

                    def gen_pro(j, t0=t0, A2=A2, S2=S2, b_af=b_af):
                        t = t0 + j
                        k.dma("sp", acc[:, j, :], xres[t * 128:(t + 1) * 128, :], reads=[b_xr[t]], writes=[b_acc[j]])
                        s4, b_s4 = s4r.next()
                        k.op("act", lambda e, j=j, s4=s4: e.activation(out=junk[:], in_=acc[:, j, :], func=AF.Square,
                                                                       accum_out=s4[:, 0:1]), reads=[b_acc[j]], writes=[b_junk, b_s4])
                        rms_rstd(s4[:, 0:1], s4[:, 1:2], D, b_s4, b_s4)
                        h2, b_h2 = h2r.next()
                        hb, b_hb = hbr.next()
                        k.op("dve", lambda e, j=j, s4=s4, h2=h2, A2=A2: e.scalar_tensor_tensor(
                            out=h2[:], in0=acc[:, j, :], scalar=s4[:, 1:2], in1=A2[:], op0=ALU.mult, op1=ALU.mult),
                            reads=[b_acc[j], b_s4, b_af], writes=[b_h2])
                        k.op("pool", lambda e, h2=h2, S2=S2: e.tensor_tensor(out=h2[:], in0=h2[:], in1=S2[:], op=ALU.add),
                             reads=[b_h2, b_af], writes=[b_h2])
                        k.op("pool", lambda e, h2=h2, hb=hb: e.tensor_copy(out=hb[:], in_=h2[:]), reads=[b_h2], writes=[b_hb])
                        yield
                        pT, b_pT = psT.next()
                        for kc in range(8):
                            k.op("pe", lambda e, kc=kc, pT=pT, hb=hb: e.transpose(out=pT[:, kc, :], in_=hb[:, kc * 128:(kc + 1) * 128],
                                                                                  identity=ident_b[:]), reads=[b_hb], writes=[b_pT])
                        k.op("act", lambda e, pT=pT, j=j: e.copy(out=h2T[:, :, j * 128:(j + 1) * 128], in_=pT[:]), reads=[b_pT],
                             writes=[b_h2T])
                        if moe:
                            for r_ in range(2):
                                pR, b_pR = psR.next()
                                for c4 in range(4):
                                    kc = r_ * 4 + c4
                                    k.op("pe", lambda e, pR=pR, c4=c4, kc=kc, h2=h2: e.transpose(
                                        out=pR[:, c4, :], in_=h2[:, kc * 128:(kc + 1) * 128], identity=ident_f[:]),
                                        reads=[b_h2], writes=[b_pR])
                                k.op("act", lambda e, pR=pR, r_=r_: e.copy(out=h2Tf[:, r_ * 4:(r_ + 1) * 4, :], in_=pR[:]),
                                     reads=[b_pR], writes=[b_h2Tf])
                            yield
                            pL, b_pL = psY.next()
                            for kc in range(8):
                                k.op("pe", lambda e, pL=pL, kc=kc: e.matmul(pL[:, 0:8], lhsT=h2Tf[:, kc, :], rhs=rtf[:, kc, :],
                                                                            start=(kc == 0), stop=(kc == 7)),
                                     reads=[b_h2Tf, b_rtf], writes=[b_pL])
                            g, b_g = g8.next()
                            k.op("dve", lambda e, g=g, pL=pL: e.tensor_copy(out=g[:, 0:8], in_=pL[:, 0:8]), reads=[b_pL], writes=[b_g])
                            k.op("dve", lambda e, g=g: e.max(out=g[:, 8:16], in_=g[:, 0:8]), reads=[b_g], writes=[b_g])
                            k.op("dve", lambda e, g=g: e.tensor_scalar(out=g[:, 16:24], in0=g[:, 0:8], scalar1=g[:, 8:9], scalar2=None,
                                                                       op0=ALU.subtract), reads=[b_g], writes=[b_g])
                            k.op("act", lambda e, g=g: e.activation(out=g[:, 16:24], in_=g[:, 16:24], func=AF.Exp), reads=[b_g],
                                 writes=[b_g])
                            k.op("dve", lambda e, g=g: e.scalar_tensor_tensor(out=g[:, 16:24], in0=g[:, 0:8], scalar=g[:, 9:10],
                                                                              in1=g[:, 16:24], op0=ALU.is_ge, op1=ALU.mult),
                                 reads=[b_g], writes=[b_g])
                            k.op("dve", lambda e, g=g: e.reduce_sum(out=g[:, 24:25], in_=g[:, 16:24], axis=AX.X), reads=[b_g],
                                 writes=[b_g])
                            k.op("dve", lambda e, g=g: e.reciprocal(out=g[:, 24:25], in_=g[:, 24:25]), reads=[b_g], writes=[b_g])
                            k.op("dve", lambda e, g=g, j=j: e.tensor_scalar(out=gates[:, j, :], in0=g[:, 16:24], scalar1=g[:, 24:25],
                                                                            scalar2=None, op0=ALU.mult), reads=[b_g],
                                 writes=[b_gates])
                    run_pipelined(gen_pro, range(ntb), 3)
                    subs = [(s0, min(4, ntb - s0)) for s0 in range(0, ntb, 4)]
                    for e_ in range(nexp):
                        wg_d, wu_d, wd_d = wsrc(e_)
                        for (c0, F) in fblocks:
                            wg, b_wg = wgr.next()
                            wu, b_wu = wur.next()
                            wd, b_wd = wdr.next()
                            for (dst, b_dst, srcw, eng) in ((wg, b_wg, wg_d, "pool"), (wu, b_wu, wu_d, "act")):
                                sg_, b_sg_ = stgr.next()
                                sv_ = sg_[:, 0:8 * F * 128].rearrange("p (c n) -> p c n", c=8)
                                k.dma("sp", sv_, srcw[:, c0 * 128:(c0 + F) * 128].rearrange("(c p) n -> p c n", p=128),
                                      writes=[b_sg_])
                                if eng == "pool":
                                    k.op("pool", lambda e, dst=dst, sv_=sv_, F=F: e.tensor_copy(out=dst[:, :, 0:F * 128], in_=sv_),
                                         reads=[b_sg_], writes=[b_dst])
                                else:
                                    k.op("act", lambda e, dst=dst, sv_=sv_, F=F: e.copy(out=dst[:, :, 0:F * 128], in_=sv_),
                                         reads=[b_sg_], writes=[b_dst])
                            sg_, b_sg_ = stgr.next()
                            sv_ = sg_[:, 0:F * D].rearrange("p (c n) -> p c n", c=F)
                            k.dma("sp", sv_, wd_d[c0 * 128:(c0 + F) * 128, :].rearrange("(c p) n -> p c n", p=128), writes=[b_sg_])
                            k.op("pool", lambda e, wd=wd, sv_=sv_, F=F, G2=G2: e.tensor_tensor(
                                out=wd[:, 0:F, :], in0=sv_, in1=G2[:].rearrange("p (o n) -> p o n", o=1).broadcast_to([128, F, D]),
                                op=ALU.mult), reads=[b_sg_, b_af], writes=[b_wd])
                            for (s0, ns) in subs:
                                N = ns * 128
                                aT, b_aT = actr.next()
                                for fc in range(F):
                                    pG, b_pG = psG.next()
                                    pU_, b_pU = psU.next()
                                    for kc in range(8):
                                        k.op("pe", lambda e, pG=pG, wg=wg, kc=kc, fc=fc, s0=s0, N=N: e.matmul(
                                            pG[:, 0:N], lhsT=wg[:, kc, fc * 128:(fc + 1) * 128], rhs=h2T[:, kc, s0 * 128:s0 * 128 + N],
                                            start=(kc == 0), stop=(kc == 7)), reads=[b_wg, b_h2T], writes=[b_pG])
                                    for kc in range(8):
                                        k.op("pe", lambda e, pU_=pU_, wu=wu, kc=kc, fc=fc, s0=s0, N=N: e.matmul(
                                            pU_[:, 0:N], lhsT=wu[:, kc, fc * 128:(fc + 1) * 128], rhs=h2T[:, kc, s0 * 128:s0 * 128 + N],
                                            start=(kc == 0), stop=(kc == 7)), reads=[b_wu, b_h2T], writes=[b_pU])
                                    sgt, b_sgt = sgr.next()
                                    k.op("act", lambda e, sgt=sgt, pG=pG, N=N: e.activation(out=sgt[:, 0:N], in_=pG[:, 0:N], func=AF.Silu),
                                         reads=[b_pG], writes=[b_sgt])
                                    k.op("dve", lambda e, aT=aT, fc=fc, sgt=sgt, pU_=pU_, N=N: e.tensor_tensor(
                                        out=aT[:, fc, 0:N], in0=pU_[:, 0:N], in1=sgt[:, 0:N], op=ALU.mult),
                                        reads=[b_pU, b_sgt], writes=[b_aT])
                                for jj in range(ns):
                                    j = s0 + jj
                                    for hf in range(2):
                                        pY, b_pY = psY.next()
                                        for fc in range(F):
                                            k.op("pe", lambda e, pY=pY, aT=aT, wd=wd, fc=fc, jj=jj, hf=hf, F=F: e.matmul(
                                                pY[:], lhsT=aT[:, fc, jj * 128:(jj + 1) * 128], rhs=wd[:, fc, hf * 512:(hf + 1) * 512],
                                                start=(fc == 0), stop=(fc == F - 1)), reads=[b_aT, b_wd], writes=[b_pY])
                                        if moe:
                                            k.op("dve", lambda e, pY=pY, j=j, hf=hf, e_=e_: e.scalar_tensor_tensor(
                                                out=acc[:, j, hf * 512:(hf + 1) * 512], in0=pY[:], scalar=gates[:, j, e_:e_ + 1],
                                                in1=acc[:, j, hf * 512:(hf + 1) * 512], op0=ALU.mult, op1=ALU.add),
                                                reads=[b_pY, b_gates, b_acc[j]], writes=[b_acc[j]])
                                        else:
                                            k.op("dve", lambda e, pY=pY, j=j, hf=hf: e.tensor_tensor(
                                                out=acc[:, j, hf * 512:(hf + 1) * 512], in0=pY[:],
                                                in1=acc[:, j, hf * 512:(hf + 1) * 512], op=ALU.add),
                                                reads=[b_pY, b_acc[j]], writes=[b_acc[j]])
                    for j in range(ntb):
                        t = t0 + j
                        if not last:
                            k.dma("act", xres[t * 128:(t + 1) * 128, :], acc[:, j, :], reads=[b_acc[j]], writes=[b_xr[t]])
                        else:
                            s4, b_s4 = s4r.next()
                            k.op("act", lambda e, j=j, s4=s4: e.activation(out=junk[:], in_=acc[:, j, :], func=AF.Square,
                                                                           accum_out=s4[:, 0:1]),
                                 reads=[b_acc[j]], writes=[b_junk, b_s4])
                            rms_rstd(s4[:, 0:1], s4[:, 1:2], D, b_s4, b_s4)
                            ot, b_ot = outr.next()
                            k.op("dve", lambda e, j=j, s4=s4, ot=ot: e.scalar_tensor_tensor(
                                out=ot[:], in0=acc[:, j, :], scalar=s4[:, 1:2], in1=gF[:], op0=ALU.mult, op1=ALU.mult),
                                reads=[b_acc[j], b_s4, b_gF], writes=[b_ot])
                            k.dma("act", out_d[t * 128:(t + 1) * 128, :], ot[:], reads=[b_ot])
                k.barrier()
            if stop_after == ("P5", layer):
                break
        k.barrier()
        k.emit()
    return nc, k


_CONSTS = None


def _in_maps(inputs):
    global _CONSTS
    if _CONSTS is None:
        _CONSTS = _consts()
    maps = []
    shared = {kk: np.ascontiguousarray(np.asarray(inputs[kk], dtype=np.float32)) for kk in W_SHAPES}
    for b in range(8):
        m = dict(shared)
        m.update(_CONSTS)
        m["x"] = np.ascontiguousarray(np.asarray(inputs["x"][b], dtype=np.float32))
        m["ctx"] = np.ascontiguousarray(np.asarray(inputs["ctx"][b], dtype=np.float32))
        m["c2"] = np.ascontiguousarray(np.stack([np.asarray(inputs["c"][b]), np.asarray(inputs["c_ctx"])]).astype(np.float32))
        maps.append(m)
    return maps


def kernel(**inputs):
    nc, _ = build()
    maps = _in_maps(inputs)
    res = run_bass_kernel_spmd(nc, maps, core_ids=list(range(8)))
    return np.stack([r["out"] for r in res.results], axis=0).astype(np.float32)
```

```python
import os
import contextlib
import numpy as np
import concourse.bass as bass
import concourse.mybir as mybir
from concourse.bass_utils import run_bass_kernel_spmd

F32 = mybir.dt.float32
BF16 = mybir.dt.bfloat16
AF = mybir.ActivationFunctionType
ALU = mybir.AluOpType
AX = mybir.AxisListType

D = 1024
L = 4096
LC = 256
NT = 34
NLT = 32
DEPTH = 2
IN_DIM = 2144
D_FF = 2816
NE = 8
D_FFE = 3584
EPS = 1e-6


class Buf:
    __slots__ = ("name", "w", "r", "excl")

    def __init__(self, name="", excl=False):
        self.name = name
        self.w = None
        self.r = {}
        self.excl = excl


class _Rec:
    def __init__(self):
        self.call = None

    def __getattr__(self, name):
        def f(*a, **kw):
            self.call = (name, a, kw)
            return self
        return f


def _bind(fn):
    r = _Rec()
    fn(r)
    name, a, kw = r.call
    return lambda eng: getattr(eng, name)(*a, **kw)


class KS:
    ENG = ("pe", "act", "dve", "pool", "sp")

    def __init__(self, nc, st, n_dma_sems=16):
        self.nc = nc
        self.st = st
        self.prog = {e: [] for e in self.ENG}
        self.sems = {}
        self.cnt = {}
        self.seen = {e: {} for e in self.ENG}
        for e in self.ENG:
            self._mksem("E_" + e)
        self.dma_q = {}
        for q in ("sp", "pool", "act"):
            keys = []
            for i in range(n_dma_sems if q == "sp" else (8 if q == "act" else 4)):
                k = "D_%s%d" % (q, i)
                self._mksem(k)
                keys.append(k)
            self.dma_q[q] = [keys, 0]
        self.n_instr = 0
        self.maxi = int(os.environ.get("DBG_MAXI", 10 ** 9))
        self.scr = st.enter_context(nc.sbuf_tensor("ks_scr", [128, 8], F32))
        self.prog["pool"].append(([], lambda eng: eng.memset(self.scr[:], 0.0), "E_pool", 1))
        self.cnt["E_pool"] += 1

    def _mksem(self, key):
        h = self.st.enter_context(self.nc.semaphore(key))
        self.sems[key] = h
        self.cnt[key] = 0

    def _dummy(self, e):
        key = "E_" + e
        self.cnt[key] += 1
        scr = self.scr
        col = {"act": 0, "dve": 1, "pool": 2}[e]
        if e == "act":
            fn = lambda eng: eng.copy(out=scr[:, col:col + 1], in_=scr[:, col + 4:col + 5])
        else:
            fn = lambda eng: eng.tensor_copy(out=scr[:, col:col + 1], in_=scr[:, col + 4:col + 5])
        self.prog[e].append(([], fn, key, 1))
        self.n_instr += 1

    def _deps(self, e, reads, writes):
        need = {}

        def add(ev):
            if ev is None:
                return
            k, v = ev
            if need.get(k, 0) < v:
                need[k] = v
        for b in reads:
            add(b.w)
            if b.excl:
                for k, v in b.r.items():
                    if k != "E_" + e:
                        add((k, v))
        for b in writes:
            add(b.w)
            for k, v in b.r.items():
                add((k, v))
        out = []
        own = "E_" + e
        for k, v in need.items():
            if k == own and (e == "pe" or e == "sp"):
                continue
            if self.seen[e].get(k, 0) >= v:
                continue
            self.seen[e][k] = v
            out.append((k, v))
        return out

    def op(self, e, fn, reads=(), writes=()):
        if self.n_instr >= self.maxi:
            return
        waits = self._deps(e, reads, writes)
        key = "E_" + e
        self.cnt[key] += 1
        val = self.cnt[key]
        self.prog[e].append((waits, _bind(fn), key, 1))
        for b in reads:
            if b.r.get(key, 0) < val:
                b.r[key] = val
        for b in writes:
            b.w = (key, val)
            b.r = {}
        self.n_instr += 1

    def dma(self, q, out, in_, reads=(), writes=(), **kw):
        if self.n_instr >= self.maxi:
            return
        keys, idx = self.dma_q[q]
        key = keys[idx % len(keys)]
        self.dma_q[q][1] = idx + 1
        waits = self._deps(q, reads, writes)
        prev = self.cnt[key]
        if prev > 0 and self.seen[q].get(key, 0) < prev:
            self.seen[q][key] = prev
            waits.append((key, prev))
        self.cnt[key] += 16
        val = self.cnt[key]

        def fn(eng, out=out, in_=in_, kw=kw):
            return eng.dma_start(out=out, in_=in_, **kw)
        self.prog[q].append((waits, fn, key, 16))
        for b in reads:
            if b.r.get(key, 0) < val:
                b.r[key] = val
        for b in writes:
            b.w = (key, val)
            b.r = {}
        self.n_instr += 1

    def barrier(self, engines=None):
        engines = engines or self.ENG
        snap = dict(self.cnt)
        for e in engines:
            waits = []
            for k, v in snap.items():
                if v == 0:
                    continue
                if k == "E_" + e and e in ("pe", "sp"):
                    continue
                if self.seen[e].get(k, 0) >= v:
                    continue
                self.seen[e][k] = v
                waits.append((k, v))
            if waits:
                self.prog[e].append((waits, None, None, 0))

    def emit(self):
        nc = self.nc
        ks = self
        with nc.Block() as block:
            def run(e):
                def body(eng):
                    for waits, fn, key, inc in ks.prog[e]:
                        for k, v in waits:
                            eng.wait_ge(ks.sems[k], v)
                        if fn is not None:
                            fn(eng).then_inc(ks.sems[key], inc)
                return body
            block.sync(run("sp"))
            block.tensor(run("pe"))
            block.scalar(run("act"))
            block.vector(run("dve"))
            block.gpsimd(run("pool"))


def run_pipelined(make_gen, items, depth):
    items = list(items)
    active = []
    nxt = 0
    while nxt < len(items) or active:
        if nxt < len(items) and len(active) < depth:
            active.append(make_gen(items[nxt]))
            nxt += 1
        for g in list(active):
            try:
                next(g)
            except StopIteration:
                active.remove(g)


class Ring:
    def __init__(self, alloc, name, n, shape, dt):
        self.tiles = [alloc("%s%d" % (name, i), shape, dt) for i in range(n)]
        excl = getattr(alloc, "__name__", "") == "ps"
        self.bufs = [Buf("%s%d" % (name, i), excl) for i in range(n)]
        self.i = 0

    def next(self):
        j = self.i % len(self.tiles)
        self.i += 1
        return self.tiles[j], self.bufs[j]


def _rope_cs(pos, dim):
    inv = (10000.0 ** (-np.arange(0, dim, 2, dtype=np.float32) / np.float32(dim))).astype(np.float32)
    ang = pos.astype(np.float32)[:, None] * inv[None, :]
    ang = np.concatenate([ang, ang], axis=-1).astype(np.float32)
    c = np.cos(ang).astype(np.float32)
    s = np.sin(ang).astype(np.float32)
    h = dim // 2
    s = np.concatenate([-s[:, :h], s[:, h:]], axis=-1)
    return c, s


def _consts():
    t = np.arange(L)
    rows, cols = t // 64, t % 64
    out = {}
    for nm, dim in (("ropeA", 32), ("ropeB", 64)):
        h = dim // 2
        cr, sr = _rope_cs(rows, h)
        cc, sc = _rope_cs(cols, h)
        c = np.concatenate([cr, cc], -1)
        s = np.concatenate([sr, sc], -1)
        out[nm + "_c"] = np.ascontiguousarray(c.reshape(NLT, 128, dim).transpose(1, 0, 2))
        out[nm + "_s"] = np.ascontiguousarray(s.reshape(NLT, 128, dim).transpose(1, 0, 2))
    c, s = _rope_cs(t, 64)
    ks_ = np.float32(64 ** -0.5)
    c2 = np.stack([c, c * ks_], 1)
    s2 = np.stack([s, s * ks_], 1)
    out["ropeC_c"] = np.ascontiguousarray(c2.reshape(NLT, 128, 2, 64).transpose(1, 0, 2, 3))
    out["ropeC_s"] = np.ascontiguousarray(s2.reshape(NLT, 128, 2, 64).transpose(1, 0, 2, 3))
    out["ident"] = np.eye(128, dtype=np.float32)
    k = np.arange(128)[:, None]
    q = np.arange(128)[None, :]
    NEG = np.float32(-30000.0)
    out["mask_prev"] = np.where(k >= q, 0.0, NEG).astype(np.float32)
    out["mask_next"] = np.where(k <= q, 0.0, NEG).astype(np.float32)
    out["ret_d1"] = np.maximum(q - k, 0).astype(np.float32)
    out["ret_i1"] = (q >= k).astype(np.float32)
    out["ret_d2"] = np.maximum(k - q, 0).astype(np.float32)
    out["ret_i2"] = (k >= q).astype(np.float32)
    out["ret_j1"] = np.broadcast_to((np.arange(128) + 1.0)[None, :], (128, 128)).astype(np.float32).copy()
    out["ret_j2"] = np.broadcast_to((128.0 - np.arange(128))[None, :], (128, 128)).astype(np.float32).copy()
    pc = np.zeros((128, 4), np.float32)
    pc[:, 0] = 127.0 - np.arange(128)
    pc[:, 1] = np.arange(128)
    pc[:, 2] = np.arange(128) + 1.0
    pc[:, 3] = 128.0 - np.arange(128)
    out["ret_pc"] = pc
    return out


CONST_SHAPES = {k: v.shape for k, v in _consts().items()}

W_SHAPES = {
    "w_mod": (DEPTH, D, 6 * D), "b_mod": (DEPTH, 6 * D), "norm1_g": (DEPTH, D), "norm2_g": (DEPTH, D),
    "w_in": (DEPTH, D, IN_DIM), "mla_q_norm": (DEPTH, 192), "mla_w_uq": (DEPTH, 192, 384),
    "mla_kv_norm": (DEPTH, 128), "mla_w_ukv": (DEPTH, 128, 512), "swa_sink": (DEPTH, 8),
    "ret_decay_fwd": (DEPTH, 4), "ret_decay_bwd": (DEPTH, 4), "w_out": (DEPTH, D, D),
    "ffn_w_gate": (1, D, D_FF), "ffn_w_up": (1, D, D_FF), "ffn_w_down": (1, D_FF, D),
    "moe_router": (1, D, NE), "moe_w_gate": (1, NE, D, D_FFE), "moe_w_up": (1, NE, D, D_FFE),
    "moe_w_down": (1, NE, D_FFE, D), "final_norm_g": (D,),
}


def build(stop_after=None, debug=False, n_layers=DEPTH):
    nc = bass.Bass("TRN2", target_bir_lowering=False)
    dkind = "ExternalOutput" if debug else "Internal"

    def din(name, shape):
        return nc.dram_tensor(name, list(shape), F32, kind="ExternalInput").ap()

    x_in = din("x", (L, D))
    ctx_in = din("ctx", (LC, D))
    c2_in = din("c2", (2, D))
    W = {k: din(k, s) for k, s in W_SHAPES.items()}
    C = {k: din(k, s) for k, s in CONST_SHAPES.items()}
    out_d = nc.dram_tensor("out", [L, D], F32, kind="ExternalOutput").ap()
    xres = nc.dram_tensor("xres", [NT * 128, D], F32, kind=dkind).ap()
    modv = nc.dram_tensor("modv", [DEPTH, 2, 6 * D], F32, kind=dkind).ap()
    hT_d = nc.dram_tensor("hT_d", [NT, 128, D], BF16, kind=dkind).ap()
    mixT_d = nc.dram_tensor("mixT_d", [8, 128, NT * 128], BF16, kind=dkind).ap()

    done = [False]

    with contextlib.ExitStack() as st0:
        k = KS(nc, st0)

        uid = [0]

        def mk_alloc(st):
            def sb(name, shape, dt):
                uid[0] += 1
                return st.enter_context(nc.sbuf_tensor("s%d_%s" % (uid[0], name), list(shape), dt))

            def ps(name, shape, dt):
                uid[0] += 1
                return st.enter_context(nc.psum_tensor("p%d_%s" % (uid[0], name), list(shape), dt))
            return sb, ps

        sb0, ps0 = mk_alloc(st0)
        ident_f = sb0("ident_f", [128, 128], F32)
        ident_b = sb0("ident_b", [128, 128], BF16)
        ones_f = sb0("ones_f", [128, 128], F32)
        b_c = Buf("consts")
        k.dma("sp", ident_f[:], C["ident"][:, :], writes=[b_c])
        k.op("dve", lambda e: e.tensor_copy(out=ident_b[:], in_=ident_f[:]), reads=[b_c], writes=[b_c])
        k.op("pool", lambda e: e.memset(ones_f[:], 1.0), writes=[b_c])
        k.barrier()

        def rms_rstd(ss_ap, rstd_ap, n, b_ss, b_r):
            k.op("dve", lambda e: e.tensor_scalar(out=rstd_ap, in0=ss_ap, scalar1=1.0 / n, scalar2=EPS,
                                                  op0=ALU.mult, op1=ALU.add), reads=[b_ss], writes=[b_r])
            k.op("act", lambda e: e.activation(out=rstd_ap, in_=rstd_ap, func=AF.Sqrt), reads=[b_r], writes=[b_r])
            k.op("dve", lambda e: e.reciprocal(out=rstd_ap, in_=rstd_ap), reads=[b_r], writes=[b_r])

        def x_src(layer, t):
            if layer == 0:
                return x_in[t * 128:(t + 1) * 128, :] if t < NLT else ctx_in[(t - NLT) * 128:(t - NLT + 1) * 128, :]
            return xres[t * 128:(t + 1) * 128, :]

        def load_rep(dst, src_row, b):
            k.dma("sp", dst, src_row.broadcast_to([128, src_row.shape[-1]]), writes=[b])

        def rope(eng, dst, src, tc_, ts_, tmp, rd, wr, b_tmp):
            k.op(eng, lambda e: e.tensor_tensor(out=tmp[:, :, :, 0, :], in0=src[:, :, :, 1, :], in1=ts_[:, :, :, 0, :],
                                                op=ALU.mult), reads=rd, writes=[b_tmp])
            k.op(eng, lambda e: e.tensor_tensor(out=tmp[:, :, :, 1, :], in0=src[:, :, :, 0, :], in1=ts_[:, :, :, 1, :],
                                                op=ALU.mult), reads=rd, writes=[b_tmp])
            k.op(eng, lambda e: e.tensor_tensor(out=dst, in0=src, in1=tc_, op=ALU.mult), reads=rd, writes=wr)
            k.op(eng, lambda e: e.tensor_tensor(out=dst, in0=dst, in1=tmp, op=ALU.add), reads=[b_tmp] + wr, writes=wr)

        for layer in range(n_layers):
            last = layer == DEPTH - 1
            ntile = NLT if last else NT

            with contextlib.ExitStack() as st:
                sb, ps = mk_alloc(st)
                c2 = sb("c2", [2, D], F32)
                c2s = sb("c2s", [2, D], F32)
                cT = sb("cT", [128, 8, 2], F32)
                bm = sb("bm", [2, 6 * D], F32)
                mo = sb("mo", [2, 6 * D], F32)
                wm = Ring(sb, "wm", 2, [128, 8, 512], F32)
                pT = ps("p0T", [128, 8, 2], F32)
                pm = Ring(ps, "p0m", 2, [2, 512], F32)
                b_c2, b_cT, b_pT, b_bm, b_mo = Buf(), Buf(), Buf(), Buf(), Buf()
                k.dma("sp", c2[:], c2_in[:, :], writes=[b_c2])
                k.dma("sp", bm[:], W["b_mod"][layer:layer + 1, :].broadcast_to([2, 6 * D]), writes=[b_bm])
                k.op("act", lambda e: e.activation(out=c2s[:], in_=c2[:], func=AF.Silu), reads=[b_c2], writes=[b_c2])
                for kc in range(8):
                    k.op("pe", lambda e, kc=kc: e.transpose(out=pT[:, kc, :], in_=c2s[:, kc * 128:(kc + 1) * 128],
                                                            identity=ident_f[0:2, 0:2]), reads=[b_c2], writes=[b_pT])
                k.op("dve", lambda e: e.tensor_copy(out=cT[:], in_=pT[:]), reads=[b_pT], writes=[b_cT])
                for cc in range(12):
                    wt, wb = wm.next()
                    k.dma("sp", wt[:], W["w_mod"][layer, :, cc * 512:(cc + 1) * 512].rearrange("(c p) n -> p c n", p=128),
                          writes=[wb])
                    pt, pb = pm.next()
                    for kc in range(8):
                        k.op("pe", lambda e, kc=kc, pt=pt, wt=wt: e.matmul(pt[:], lhsT=cT[:, kc, :], rhs=wt[:, kc, :],
                                                                           start=(kc == 0), stop=(kc == 7)),
                             reads=[b_cT, wb], writes=[pb])
                    k.op("dve", lambda e, pt=pt, cc=cc: e.tensor_tensor(out=mo[:, cc * 512:(cc + 1) * 512], in0=pt[:],
                                                                        in1=bm[:, cc * 512:(cc + 1) * 512], op=ALU.add),
                         reads=[pb, b_bm], writes=[b_mo])
                k.dma("sp", modv[layer], mo[:], reads=[b_mo])
                k.barrier()
            if stop_after == ("P0", layer):
                break

            def mod_row(which, idx):
                return modv[layer, which:which + 1, idx * D:(idx + 1) * D]

            def load_affine(sb, name, which, norm_g, i_sc, i_sh):
                A = sb(name + "A", [128, D], F32)
                SH = sb(name + "S", [128, D], F32)
                b = Buf()
                with contextlib.ExitStack() as stg_:
                    G = stg_.enter_context(nc.sbuf_tensor("%s_G%d" % (name, layer), [128, D], F32))
                    load_rep(A[:], mod_row(which, i_sc), b)
                    load_rep(SH[:], mod_row(which, i_sh), b)
                    load_rep(G[:], norm_g[layer:layer + 1, :], b)
                    k.op("dve", lambda e: e.scalar_tensor_tensor(out=A[:], in0=A[:], scalar=1.0, in1=G[:], op0=ALU.add,
                                                                 op1=ALU.mult), reads=[b], writes=[b])
                    k.barrier()
                return A, SH, b

            with contextlib.ExitStack() as st:
                sb, ps = mk_alloc(st)
                AL, SL, b_afl = load_affine(sb, "n1l", 0, W["norm1_g"], 1, 0)
                AC, SC, b_afc = load_affine(sb, "n1c", 1, W["norm1_g"], 1, 0)
                winA = sb("winA", [128, 8, 352], BF16)
                wuq0 = sb("wuq0", [128, 384], BF16)
                wuq1 = sb("wuq1", [64, 384], BF16)
                wukv = sb("wukv", [128, 512], BF16)
                b_w = Buf()
                with contextlib.ExitStack() as stw:
                    sbw, _ = mk_alloc(stw)
                    stg = sbw("stgA", [128, 8, 352], F32)
                    s1 = sbw("stg1", [128, 512], F32)
                    s2 = sbw("stg2", [64, 384], F32)
                    s3 = sbw("stg3", [128, 512], F32)
                    gq = sbw("gq", [128, 2], F32)
                    gkv = sbw("gkv", [128, 1], F32)
                    b_s = Buf()
                    k.dma("sp", stg[:], W["w_in"][layer, :, 0:352].rearrange("(c p) n -> p c n", p=128), writes=[b_s])
                    k.dma("sp", s1[:, 0:384], W["mla_w_uq"][layer, 0:128, :], writes=[b_s])
                    k.dma("sp", s2[:], W["mla_w_uq"][layer, 128:192, :], writes=[b_s])
                    k.dma("sp", s3[:], W["mla_w_ukv"][layer, :, :], writes=[b_s])
                    k.dma("sp", gq[:, 0:1], W["mla_q_norm"][layer, 0:128].rearrange("(p o) -> p o", o=1), writes=[b_s])
                    k.dma("sp", gq[0:64, 1:2], W["mla_q_norm"][layer, 128:192].rearrange("(p o) -> p o", o=1), writes=[b_s])
                    k.dma("sp", gkv[:], W["mla_kv_norm"][layer, :].rearrange("(p o) -> p o", o=1), writes=[b_s])
                    k.op("pool", lambda e: e.tensor_copy(out=winA[:], in_=stg[:]), reads=[b_s], writes=[b_w])
                    k.op("dve", lambda e: e.tensor_scalar(out=wuq0[:], in0=s1[:, 0:384], scalar1=gq[:, 0:1], scalar2=None,
                                                          op0=ALU.mult), reads=[b_s], writes=[b_w])
                    k.op("dve", lambda e: e.tensor_scalar(out=wuq1[:], in0=s2[:], scalar1=gq[0:64, 1:2], scalar2=None,
                                                          op0=ALU.mult), reads=[b_s], writes=[b_w])
                    k.op("dve", lambda e: e.tensor_scalar(out=wukv[:], in0=s3[:], scalar1=gkv[:, 0:1], scalar2=None,
                                                          op0=ALU.mult), reads=[b_s], writes=[b_w])
                    k.barrier()
                rc = sb("ropeAc", [128, NLT, 32], F32)
                rs = sb("ropeAs", [128, NLT, 32], F32)
                b_rt = Buf()
                k.dma("sp", rc[:], C["ropeA_c"][:, :, :], writes=[b_rt])
                k.dma("sp", rs[:], C["ropeA_s"][:, :, :], writes=[b_rt])
                QT = sb("QTa", [96, 4, NT * 128], BF16)
                KT = sb("KTa", [96, 4, NT * 128], BF16)
                VA = sb("VA", [128, NT, 2, 192], BF16)
                b_QT, b_KT, b_VA = Buf(), Buf(), Buf()
                k.op("pool", lambda e: e.memset(VA[:], 0.0), writes=[b_VA])
                k.op("pool", lambda e: e.memset(VA[:, :, :, 64:65], 1.0), writes=[b_VA])
                k.barrier()
                with contextlib.ExitStack() as stp:
                    sbp, psp = mk_alloc(stp)
                    xr = Ring(sbp, "xt", 3, [128, D], F32)
                    junk = sbp("junk", [128, D], BF16)
                    b_junk = Buf()
                    h1r = Ring(sbp, "h1", 2, [128, D], F32)
                    hbr = Ring(sbp, "hb", 3, [128, D], BF16)
                    hTr = Ring(sbp, "hT", 3, [128, 8, 128], BF16)
                    st4 = Ring(sbp, "st4", 3, [128, 8], F32)
                    cnr = Ring(sbp, "cn", 3, [128, 320], BF16)
                    cTr = Ring(sbp, "cTs", 3, [128, 3, 128], BF16)
                    kper = Ring(sbp, "kpe", 3, [128, 32], F32)
                    tmpr = Ring(sbp, "rtmp", 3, [128, 4, 32], F32)
                    qfr = Ring(sbp, "qf", 3, [128, 4, 96], BF16)
                    qsr = Ring(sbp, "qs", 3, [128, 384], F32)
                    kfr = Ring(sbp, "kf", 3, [128, 4, 96], BF16)
                    pTr = Ring(psp, "pT", 2, [128, 8, 128], BF16)
                    pAr = Ring(psp, "pA", 2, [128, 512], F32)
                    pq = Ring(psp, "pq", 1, [128, 512], F32)
                    pkv = Ring(psp, "pkv", 1, [128, 512], F32)
                    pT2 = Ring(psp, "pT2", 2, [128, 8, 128], BF16)
                    v5 = lambda a: a.rearrange("p h (g j e) -> p h g j e", g=2, j=2)

                    def gen1(t):
                        isc = t >= NLT
                        A_, S_, b_af = (AC, SC, b_afc) if isc else (AL, SL, b_afl)
                        xt, b_x = xr.next()
                        k.dma("sp", xt[:], x_src(layer, t), writes=[b_x])
                        if layer == 0:
                            k.dma("act", xres[t * 128:(t + 1) * 128, :], xt[:], reads=[b_x])
                        s4, b_s4 = st4.next()
                        k.op("act", lambda e, xt=xt, s4=s4: e.activation(out=junk[:], in_=xt[:], func=AF.Square,
                                                                         accum_out=s4[:, 0:1]),
                             reads=[b_x], writes=[b_junk, b_s4])
                        rms_rstd(s4[:, 0:1], s4[:, 1:2], D, b_s4, b_s4)
                        h1, b_h1 = h1r.next()
                        hb, b_hb = hbr.next()
                        k.op("dve", lambda e, xt=xt, s4=s4, h1=h1, A_=A_: e.scalar_tensor_tensor(
                            out=h1[:], in0=xt[:], scalar=s4[:, 1:2], in1=A_[:], op0=ALU.mult, op1=ALU.mult),
                            reads=[b_x, b_s4, b_af], writes=[b_h1])
                        k.op("pool", lambda e, h1=h1, hb=hb, S_=S_: e.tensor_tensor(out=hb[:], in0=h1[:], in1=S_[:],
                                                                                    op=ALU.add),
                             reads=[b_h1, b_af], writes=[b_hb])
                        pT, b_pT = pTr.next()
                        for kc in range(8):
                            k.op("pe", lambda e, kc=kc, pT=pT, hb=hb: e.transpose(out=pT[:, kc, :],
                                                                                  in_=hb[:, kc * 128:(kc + 1) * 128],
                                                                                  identity=ident_b[:]),
                                 reads=[b_hb], writes=[b_pT])
                        hT, b_hT = hTr.next()
                        k.op("act", lambda e, hT=hT, pT=pT: e.copy(out=hT[:], in_=pT[:]), reads=[b_pT], writes=[b_hT])
                        k.dma("act", hT_d[t], hT[:].rearrange("p c n -> p (c n)"), reads=[b_hT])
                        yield
                        pA, b_pA = pAr.next()
                        for kc in range(8):
                            k.op("pe", lambda e, kc=kc, pA=pA, hT=hT: e.matmul(pA[:, 0:352], lhsT=hT[:, kc, :],
                                                                               rhs=winA[:, kc, :], start=(kc == 0),
                                                                               stop=(kc == 7)),
                                 reads=[b_hT, b_w], writes=[b_pA])
                        yield
                        k.op("act", lambda e, pA=pA, s4=s4: e.activation(out=junk[:, 0:192], in_=pA[:, 0:192], func=AF.Square,
                                                                         accum_out=s4[:, 2:3]),
                             reads=[b_pA], writes=[b_junk, b_s4])
                        k.op("act", lambda e, pA=pA, s4=s4: e.activation(out=junk[:, 192:320], in_=pA[:, 192:320],
                                                                         func=AF.Square, accum_out=s4[:, 3:4]),
                             reads=[b_pA], writes=[b_junk, b_s4])
                        rms_rstd(s4[:, 2:3], s4[:, 4:5], 192, b_s4, b_s4)
                        rms_rstd(s4[:, 3:4], s4[:, 5:6], 128, b_s4, b_s4)
                        cn, b_cn = cnr.next()
                        k.op("dve", lambda e, cn=cn, pA=pA, s4=s4: e.tensor_scalar(out=cn[:, 0:192], in0=pA[:, 0:192],
                                                                                   scalar1=s4[:, 4:5], scalar2=None,
                                                                                   op0=ALU.mult),
                             reads=[b_pA, b_s4], writes=[b_cn])
                        k.op("dve", lambda e, cn=cn, pA=pA, s4=s4: e.tensor_scalar(out=cn[:, 192:320], in0=pA[:, 192:320],
                                                                                   scalar1=s4[:, 5:6], scalar2=None,
                                                                                   op0=ALU.mult),
                             reads=[b_pA, b_s4], writes=[b_cn])
                        kpe, b_kpe = kper.next()
                        tmp, b_tmp = tmpr.next()
                        if isc:
                            k.op("act", lambda e, kpe=kpe, pA=pA: e.copy(out=kpe[:], in_=pA[:, 320:352]),
                                 reads=[b_pA], writes=[b_kpe])
                        else:
                            src = v5(pA[:, 320:352].rearrange("p (h n) -> p h n", h=1))
                            dst = v5(kpe[:].rearrange("p (h n) -> p h n", h=1))
                            tc_ = v5(rc[:, t:t + 1, :])
                            ts_ = v5(rs[:, t:t + 1, :])
                            tm = v5(tmp[:, 0:1, :])
                            rope("dve", dst, src, tc_, ts_, tm, [b_pA, b_rt], [b_kpe], b_tmp)
                        p2, b_p2 = pT2.next()
                        k.op("pe", lambda e, p2=p2, cn=cn: e.transpose(out=p2[:, 0, :], in_=cn[:, 0:128], identity=ident_b[:]),
                             reads=[b_cn], writes=[b_p2])
                        k.op("pe", lambda e, p2=p2, cn=cn: e.transpose(out=p2[0:64, 1, :], in_=cn[:, 128:192],
                                                                       identity=ident_b[:]),
                             reads=[b_cn], writes=[b_p2])
                        k.op("pe", lambda e, p2=p2, cn=cn: e.transpose(out=p2[:, 2, :], in_=cn[:, 192:320],
                                                                       identity=ident_b[:]),
                             reads=[b_cn], writes=[b_p2])
                        cTs, b_cT = cTr.next()
                        k.op("act", lambda e, cTs=cTs, p2=p2: e.copy(out=cTs[:, 0, :], in_=p2[:, 0, :]), reads=[b_p2],
                             writes=[b_cT])
                        k.op("act", lambda e, cTs=cTs, p2=p2: e.copy(out=cTs[0:64, 1, :], in_=p2[0:64, 1, :]), reads=[b_p2],
                             writes=[b_cT])
                        k.op("act", lambda e, cTs=cTs, p2=p2: e.copy(out=cTs[:, 2, :], in_=p2[:, 2, :]), reads=[b_p2],
                             writes=[b_cT])
                        need_q = (not isc) or (not last)
                        yield
                        pq_t, b_pq = pq.next()
                        pkv_t, b_pkv = pkv.next()
                        if need_q:
                            k.op("pe", lambda e, pq_t=pq_t, cTs=cTs: e.matmul(pq_t[:, 0:384], lhsT=cTs[:, 0, :], rhs=wuq0[:],
                                                                              start=True, stop=False),
                                 reads=[b_cT, b_w], writes=[b_pq])
                            k.op("pe", lambda e, pq_t=pq_t, cTs=cTs: e.matmul(pq_t[:, 0:384], lhsT=cTs[0:64, 1, :],
                                                                              rhs=wuq1[:], start=False, stop=True),
                                 reads=[b_cT, b_w], writes=[b_pq])
                        k.op("pe", lambda e, pkv_t=pkv_t, cTs=cTs: e.matmul(pkv_t[:], lhsT=cTs[:, 2, :], rhs=wukv[:],
                                                                            start=True, stop=True),
                             reads=[b_cT, b_w], writes=[b_pkv])
                        yield
                        qf, b_qf = qfr.next()
                        kf, b_kf = kfr.next()
                        pkv3 = pkv_t[:].rearrange("p (h n) -> p h n", h=4)
                        if need_q:
                            qs, b_qs = qsr.next()
                            k.op("act", lambda e, qs=qs, pq_t=pq_t: e.copy(out=qs[:], in_=pq_t[:, 0:384]),
                                 reads=[b_pq], writes=[b_qs])
                            qs3 = qs[:].rearrange("p (h n) -> p h n", h=4)
                            k.op("pool", lambda e, qf=qf, qs3=qs3: e.tensor_copy(out=qf[:, :, 0:64], in_=qs3[:, :, 0:64]),
                                 reads=[b_qs], writes=[b_qf])
                            if isc:
                                k.op("pool", lambda e, qf=qf, qs3=qs3: e.tensor_copy(out=qf[:, :, 64:96], in_=qs3[:, :, 64:96]),
                                     reads=[b_qs], writes=[b_qf])
                            else:
                                src = v5(qs3[:, :, 64:96])
                                dst = v5(qf[:, :, 64:96])
                                tc_ = v5(rc[:, t:t + 1, :].broadcast_to([128, 4, 32]))
                                ts_ = v5(rs[:, t:t + 1, :].broadcast_to([128, 4, 32]))
                                tm = v5(tmp[:])
                                rope("dve", dst, src, tc_, ts_, tm, [b_qs, b_rt], [b_qf], b_tmp)
                        k.op("act", lambda e, kf=kf, pkv3=pkv3: e.copy(out=kf[:, :, 0:64], in_=pkv3[:, :, 0:64]),
                             reads=[b_pkv], writes=[b_kf])
                        k.op("pool", lambda e, kf=kf, kpe=kpe: e.tensor_copy(
                            out=kf[:, :, 64:96], in_=kpe[:].rearrange("p (h n) -> p h n", h=1).broadcast_to([128, 4, 32])),
                            reads=[b_kpe], writes=[b_kf])
                        pkv4 = pkv_t[:].rearrange("p (a j n) -> p a j n", a=2, j=2)
                        k.op("act", lambda e, pkv4=pkv4, t=t: e.copy(out=VA[:, t, :, 0:64], in_=pkv4[:, :, 0, 64:128]),
                             reads=[b_pkv], writes=[b_VA])
                        k.op("act", lambda e, pkv4=pkv4, t=t: e.copy(out=VA[:, t, :, 128:192], in_=pkv4[:, :, 1, 64:128]),
                             reads=[b_pkv], writes=[b_VA])
                        p3, b_p3 = pTr.next()
                        for h in range(4):
                            if need_q:
                                k.op("pe", lambda e, h=h, p3=p3, qf=qf: e.transpose(out=p3[0:96, h, :], in_=qf[:, h, :],
                                                                                    identity=ident_b[:]),
                                     reads=[b_qf], writes=[b_p3])
                            k.op("pe", lambda e, h=h, p3=p3, kf=kf: e.transpose(out=p3[0:96, 4 + h, :], in_=kf[:, h, :],
                                                                                identity=ident_b[:]),
                                 reads=[b_kf], writes=[b_p3])
                        if need_q:
                            k.op("dve", lambda e, p3=p3, t=t: e.tensor_copy(out=QT[:, :, t * 128:(t + 1) * 128],
                                                                            in_=p3[0:96, 0:4, :]), reads=[b_p3], writes=[b_QT])
                        k.op("act", lambda e, p3=p3, t=t: e.copy(out=KT[:, :, t * 128:(t + 1) * 128], in_=p3[0:96, 4:8, :]),
                             reads=[b_p3], writes=[b_KT])
                    run_pipelined(gen1, range(NT), 4)
                    k.barrier()
                if stop_after == ("P1a", layer):
                    break
                with contextlib.ExitStack() as stp:
                    sbp, psp = mk_alloc(stp)
                    pS = Ring(psp, "pS", 4, [128, 512], F32)
                    pO = Ring(psp, "pO", 2, [128, 512], F32)
                    pB = Ring(psp, "pB", 2, [128, 512], F32)
                    PTr = Ring(sbp, "PT", 4, [128, 512], BF16)
                    recr = Ring(sbp, "rec", 2, [128, 512], F32)
                    bcr = Ring(sbp, "bcs", 2, [128, 512], F32)
                    ostg = Ring(sbp, "ostg", 3, [128, 512], BF16)
                    scale = float(96 ** -0.5)
                    qchunks = [(q0, 512, list(range(NT))) for q0 in range(0, L, 512)]
                    if not last:
                        qchunks.append((L, LC, [NLT, NLT + 1]))
                    stg_d = {}

                    def gen_mla(u):
                        if True:
                            q0, N, ktl, h = u
                            pr, odd = h // 2, h % 2
                            if not odd:
                                stg_d[(q0, pr)] = ostg.next()
                            og, b_og = stg_d[(q0, pr)]
                            po, b_po = pO.next()
                            Mlo, Mhi = (64, 192) if odd else (0, 65)
                            pend = []

                            def issue_s(kt, h=h, q0=q0, N=N):
                                pS_t, b_pS = pS.next()
                                k.op("pe", lambda e: e.matmul(pS_t[:, 0:N], lhsT=KT[:, h, kt * 128:(kt + 1) * 128],
                                                              rhs=QT[:, h, q0:q0 + N], start=True, stop=True),
                                     reads=[b_QT, b_KT], writes=[b_pS])
                                pt, b_pt = PTr.next()
                                k.op("act", lambda e: e.activation(out=pt[:, 0:N], in_=pS_t[:, 0:N], func=AF.Exp, scale=scale),
                                     reads=[b_pS], writes=[b_pt])
                                return (kt, pt, b_pt)

                            def issue_o(item, first, lastk, h=h, N=N, po=po, b_po=b_po, pr=pr, Mlo=Mlo, Mhi=Mhi):
                                kt, pt, b_pt = item
                                k.op("pe", lambda e: e.matmul(po[0:Mhi - Mlo, 0:N], lhsT=VA[:, kt, pr, Mlo:Mhi], rhs=pt[:, 0:N],
                                                              start=first, stop=lastk),
                                     reads=[b_pt, b_VA], writes=[b_po])
                            nk = len(ktl)
                            for i, kt in enumerate(ktl):
                                pend.append(issue_s(kt))
                                if len(pend) > 2:
                                    it = pend.pop(0)
                                    issue_o(it, it[0] == ktl[0], False)
                            while pend:
                                it = pend.pop(0)
                                issue_o(it, it[0] == ktl[0], len(pend) == 0)
                            yield
                            pd = 0 if odd else 64
                            pout = 64 if odd else 0
                            rec, b_rec = recr.next()
                            k.op("dve", lambda e, rec=rec, po=po, pd=pd, N=N: e.reciprocal(out=rec[pd:pd + 1, 0:N],
                                                                                           in_=po[pd:pd + 1, 0:N]),
                                 reads=[b_po], writes=[b_rec])
                            pb_t, b_pb = pB.next()
                            k.op("pe", lambda e, pb_t=pb_t, rec=rec, pd=pd, N=N: e.matmul(pb_t[:, 0:N],
                                                                                          lhsT=ones_f[pd:pd + 1, :],
                                                                                          rhs=rec[pd:pd + 1, 0:N],
                                                                                          start=True, stop=True),
                                 reads=[b_rec], writes=[b_pb])
                            bcs, b_bcs = bcr.next()
                            k.op("act", lambda e, bcs=bcs, pb_t=pb_t, pout=pout, N=N: e.copy(out=bcs[pout:pout + 64, 0:N],
                                                                                           in_=pb_t[pout:pout + 64, 0:N]),
                                 reads=[b_pb], writes=[b_bcs])
                            k.op("dve", lambda e, og=og, po=po, bcs=bcs, pout=pout, N=N: e.tensor_tensor(
                                out=og[pout:pout + 64, 0:N], in0=po[pout:pout + 64, 0:N], in1=bcs[pout:pout + 64, 0:N],
                                op=ALU.mult), reads=[b_po, b_bcs], writes=[b_og])
                            if odd:
                                k.dma("sp", mixT_d[pr, :, q0:q0 + N], og[:, 0:N], reads=[b_og])
                    run_pipelined(gen_mla, [(q0, N, ktl, h) for (q0, N, ktl) in qchunks for h in range(4)], 2)
                    k.barrier()
            if stop_after == ("P1", layer):
                break

            with contextlib.ExitStack() as st:
                sb, ps = mk_alloc(st)
                winB = sb("winB", [128, 8, 768], BF16)
                rcB = sb("ropeBc", [128, NLT, 64], F32)
                rsB = sb("ropeBs", [128, NLT, 64], F32)
                mprev = sb("mprev", [128, 512], BF16)
                mnext = sb("mnext", [128, 512], BF16)
                esink = sb("esink", [128, 2, 512], F32)
                QKB = sb("QKB", [128, 8, NT * 128], BF16)
                VB = sb("VB", [128, NT, 2, 192], BF16)
                b_w, b_rt, b_QKB, b_VB, b_es = Buf(), Buf(), Buf(), Buf(), Buf()
                with contextlib.ExitStack() as stw:
                    sbw, _ = mk_alloc(stw)
                    b_s = Buf()
                    for hf in range(2):
                        stg = sbw("stgB%d" % hf, [128, 8, 384], F32)
                        k.dma("sp", stg[:], W["w_in"][layer, :, 352 + hf * 384:352 + (hf + 1) * 384].rearrange(
                            "(c p) n -> p c n", p=128), writes=[b_s])
                        k.op("pool" if hf else "dve", lambda e, stg=stg, hf=hf: e.tensor_copy(
                            out=winB[:, :, hf * 384:(hf + 1) * 384], in_=stg[:]), reads=[b_s], writes=[b_w])
                    mf = sbw("mf", [128, 2, 128], F32)
                    k.dma("sp", mf[:, 0, :], C["mask_prev"][:, :], writes=[b_s])
                    k.dma("sp", mf[:, 1, :], C["mask_next"][:, :], writes=[b_s])
                    k.op("dve", lambda e: e.tensor_copy(out=mprev[:].rearrange("p (r n) -> p r n", r=4),
                                                        in_=mf[:, 0:1, :].broadcast_to([128, 4, 128])), reads=[b_s], writes=[b_w])
                    k.op("dve", lambda e: e.tensor_copy(out=mnext[:].rearrange("p (r n) -> p r n", r=4),
                                                        in_=mf[:, 1:2, :].broadcast_to([128, 4, 128])), reads=[b_s], writes=[b_w])
                    esk = sbw("esk", [128, 8], F32)
                    k.dma("sp", esk[:], W["swa_sink"][layer:layer + 1, :].broadcast_to([128, 8]), writes=[b_s])
                    k.op("act", lambda e: e.activation(out=esk[:], in_=esk[:], func=AF.Exp), reads=[b_s], writes=[b_s])
                    for g in range(2):
                        for half in range(2):
                            for sl in range(2):
                                h = 4 * g + 2 * sl + half
                                o0 = half * 256 + sl * 128
                                k.op("dve", lambda e, g=g, o0=o0, h=h: e.tensor_copy(
                                    out=esink[:, g, o0:o0 + 128], in_=esk[:, h:h + 1].broadcast_to([128, 128])),
                                    reads=[b_s], writes=[b_es])
                    k.dma("sp", rcB[:], C["ropeB_c"][:, :, :], writes=[b_rt])
                    k.dma("sp", rsB[:], C["ropeB_s"][:, :, :], writes=[b_rt])
                    k.op("pool", lambda e: e.memset(VB[:], 0.0), writes=[b_VB])
                    k.op("pool", lambda e: e.memset(VB[:, :, :, 64:65], 1.0), writes=[b_VB])
                    k.barrier()
                with contextlib.ExitStack() as stp:
                    sbp, psp = mk_alloc(stp)
                    hTr = Ring(sbp, "hTb", 3, [128, 8, 128], BF16)
                    qsr = Ring(sbp, "qsb", 3, [128, 512], F32)
                    ksr = Ring(sbp, "ksb", 3, [128, 256], F32)
                    tmpr = Ring(sbp, "rtmpb", 3, [128, 8, 64], F32)
                    qbr = Ring(sbp, "qb", 3, [128, 8, 64], BF16)
                    kdr = Ring(sbp, "kd", 3, [128, 2, 2, 128], BF16)
                    for kt_, kb_ in zip(kdr.tiles, kdr.bufs):
                        k.op("pool", lambda e, kt_=kt_: e.memset(kt_[:], 0.0), writes=[kb_])
                    p1r = Ring(psp, "pB1", 2, [128, 512], F32)
                    p2r = Ring(psp, "pB2", 2, [128, 512], F32)
                    pTr = Ring(psp, "pTb", 2, [128, 8, 128], BF16)
                    v5 = lambda a: a.rearrange("p h (g j e) -> p h g j e", g=2, j=2)
                    def gen2(t):
                        isc = t >= NLT
                        hT, b_hT = hTr.next()
                        k.dma("sp", hT[:].rearrange("p c n -> p (c n)"), hT_d[t], writes=[b_hT])
                        p1, b_p1 = p1r.next()
                        p2, b_p2 = p2r.next()
                        need_q = (not isc) or (not last)
                        for kc in range(8):
                            if need_q:
                                k.op("pe", lambda e, kc=kc, p1=p1, hT=hT: e.matmul(p1[:], lhsT=hT[:, kc, :], rhs=winB[:, kc, 0:512],
                                                                                   start=(kc == 0), stop=(kc == 7)),
                                     reads=[b_hT, b_w], writes=[b_p1])
                        for kc in range(8):
                            k.op("pe", lambda e, kc=kc, p2=p2, hT=hT: e.matmul(p2[:, 0:256], lhsT=hT[:, kc, :],
                                                                               rhs=winB[:, kc, 512:768], start=(kc == 0),
                                                                               stop=(kc == 7)),
                                 reads=[b_hT, b_w], writes=[b_p2])
                        yield
                        qs, b_qs = qsr.next()
                        ksb, b_ks = ksr.next()
                        tmp, b_tmp = tmpr.next()
                        qb, b_qb = qbr.next()
                        kd, b_kd = kdr.next()
                        if need_q:
                            k.op("act", lambda e, qs=qs, p1=p1: e.copy(out=qs[:], in_=p1[:]), reads=[b_p1], writes=[b_qs])
                        k.op("act", lambda e, ksb=ksb, p2=p2: e.copy(out=ksb[:], in_=p2[:, 0:256]), reads=[b_p2], writes=[b_ks])
                        qs3 = qs[:].rearrange("p (h n) -> p h n", h=8)
                        ks3 = ksb[:, 0:128].rearrange("p (h n) -> p h n", h=2)
                        if isc:
                            if need_q:
                                k.op("pool", lambda e, qb=qb, qs3=qs3: e.tensor_copy(out=qb[:], in_=qs3), reads=[b_qs], writes=[b_qb])
                            k.op("pool", lambda e, kd=kd, ks3=ks3: e.tensor_copy(out=kd[:, :, 0, 0:64], in_=ks3), reads=[b_ks],
                                 writes=[b_kd])
                        else:
                            tcq = v5(rcB[:, t:t + 1, :].broadcast_to([128, 8, 64]))
                            tsq = v5(rsB[:, t:t + 1, :].broadcast_to([128, 8, 64]))
                            rope("dve", v5(qb[:]), v5(qs3), tcq, tsq, v5(tmp[:]), [b_qs, b_rt], [b_qb], b_tmp)
                            tck = v5(rcB[:, t:t + 1, :].broadcast_to([128, 2, 64]))
                            tsk = v5(rsB[:, t:t + 1, :].broadcast_to([128, 2, 64]))
                            rope("pool", v5(kd[:, :, 0, 0:64]), v5(ks3), tck, tsk, v5(tmp[:, 0:2, :]), [b_ks, b_rt, b_qb], [b_kd],
                                 b_tmp)
                        k.op("pool", lambda e, kd=kd: e.tensor_copy(out=kd[:, :, 1, 64:128], in_=kd[:, :, 0, 0:64]), reads=[b_kd],
                             writes=[b_kd])
                        sv3 = ksb[:, 128:256].rearrange("p (g n) -> p g n", g=2)
                        k.op("pool", lambda e, sv3=sv3, t=t: e.tensor_copy(out=VB[:, t, :, 0:64], in_=sv3), reads=[b_ks],
                             writes=[b_VB])
                        k.op("pool", lambda e, sv3=sv3, t=t: e.tensor_copy(out=VB[:, t, :, 128:192], in_=sv3), reads=[b_ks],
                             writes=[b_VB])
                        yield
                        pT, b_pT = pTr.next()
                        if need_q:
                            for p in range(4):
                                k.op("pe", lambda e, p=p, pT=pT, qb=qb: e.transpose(
                                    out=pT[:, p, :], in_=qb[:, 2 * p:2 * p + 2, :].rearrange("p h n -> p (h n)"),
                                    identity=ident_b[:]), reads=[b_qb], writes=[b_pT])
                        for g in range(2):
                            for v_ in range(2):
                                k.op("pe", lambda e, g=g, v_=v_, pT=pT, kd=kd: e.transpose(
                                    out=pT[:, 4 + 2 * g + v_, :], in_=kd[:, g, v_, :], identity=ident_b[:]),
                                    reads=[b_kd], writes=[b_pT])
                        if need_q:
                            k.op("dve", lambda e, pT=pT, t=t: e.tensor_copy(out=QKB[:, :, t * 128:(t + 1) * 128], in_=pT[:, 0:8, :]),
                                 reads=[b_pT], writes=[b_QKB])
                        else:
                            k.op("dve", lambda e, pT=pT, t=t: e.tensor_copy(out=QKB[:, 4:8, t * 128:(t + 1) * 128],
                                                                            in_=pT[:, 4:8, :]), reads=[b_pT], writes=[b_QKB])
                    run_pipelined(gen2, range(NT), 3)
                    k.barrier()
                if stop_after == ("P2a", layer):
                    break
                with contextlib.ExitStack() as stp:
                    sbp, psp = mk_alloc(stp)
                    pS = Ring(psp, "pSb", 4, [128, 512], F32)
                    pO = Ring(psp, "pOb", 2, [128, 512], F32)
                    pB = Ring(psp, "pBb", 2, [128, 512], F32)
                    PTr = Ring(sbp, "PTb", 4, [128, 512], BF16)
                    recr = Ring(sbp, "recb", 2, [128, 512], F32)
                    bcr = Ring(sbp, "bcsb", 2, [128, 512], F32)
                    stg_r = [Ring(sbp, "ostb%d" % g, 2, [128, 4, 512], BF16) for g in range(2)]
                    blocks = list(range(NLT)) + ([] if last else [NLT, NLT + 1])
                    cur = [None, None]
                    def gen_swa(u):
                        i, g = u
                        isc = i >= NLT
                        if isc:
                            ktl = [(NLT, None), (NLT + 1, None)]
                            base, off, span = NLT, (i - NLT) * 128, 256
                        else:
                            ktl = ([(i - 1, mprev)] if i > 0 else []) + [(i, None)] + ([(i + 1, mnext)] if i < NLT - 1 else []) \
                                + [(NLT, None), (NLT + 1, None)]
                            base, off, span = (i // 4) * 4, (i % 4) * 128, 512
                        if True:
                            if off == 0:
                                cur[g] = stg_r[g].next()
                            og, b_og = cur[g]
                            po, b_po = pO.next()
                            pend = []

                            def pv(item, lastk, po=po, b_po=b_po, g=g):
                                kt2, pt2, b_pt2, n2 = item
                                k.op("pe", lambda e: e.matmul(po[0:65, :], lhsT=VB[:, kt2, g, 0:65], rhs=pt2[:], start=(n2 == 0),
                                                              stop=lastk), reads=[b_pt2, b_VB], writes=[b_po])
                            for n_, (kt, msk) in enumerate(ktl):
                                pS_t, b_pS = pS.next()
                                if msk is not None:
                                    k.op("pe", lambda e, pS_t=pS_t, msk=msk: e.matmul(pS_t[:], lhsT=ident_b[:], rhs=msk[:], start=True,
                                                                                      stop=False, skip_group_check=True),
                                         reads=[b_w], writes=[b_pS])
                                for half in range(2):
                                    k.op("pe", lambda e, pS_t=pS_t, half=half, kt=kt, g=g, i=i, msk=msk: e.matmul(
                                        pS_t[:, half * 256:(half + 1) * 256],
                                        lhsT=QKB[:, 4 + 2 * g + half, kt * 128:(kt + 1) * 128],
                                        rhs=QKB[:, 2 * g:2 * g + 2, i * 128:(i + 1) * 128],
                                        start=(msk is None), stop=True, skip_group_check=True),
                                        reads=[b_QKB], writes=[b_pS])
                                pt, b_pt = PTr.next()
                                k.op("act", lambda e, pt=pt, pS_t=pS_t: e.activation(out=pt[:], in_=pS_t[:], func=AF.Exp, scale=0.125),
                                     reads=[b_pS], writes=[b_pt])
                                pend.append((kt, pt, b_pt, n_))
                                if len(pend) > 1:
                                    pv(pend.pop(0), False)
                                yield
                            pv(pend.pop(0), True)
                            yield
                            rec, b_rec = recr.next()
                            k.op("dve", lambda e, rec=rec, po=po, g=g: e.tensor_tensor(
                                out=rec[64:65, :], in0=po[64:65, :], in1=esink[64:65, g, :], op=ALU.add),
                                reads=[b_po, b_es], writes=[b_rec])
                            k.op("dve", lambda e, rec=rec: e.reciprocal(out=rec[64:65, :], in_=rec[64:65, :]),
                                 reads=[b_rec], writes=[b_rec])
                            pb_t, b_pb = pB.next()
                            k.op("pe", lambda e, pb_t=pb_t, rec=rec: e.matmul(pb_t[:], lhsT=ones_f[64:65, :], rhs=rec[64:65, :],
                                                                              start=True, stop=True), reads=[b_rec], writes=[b_pb])
                            bcs, b_bcs = bcr.next()
                            k.op("act", lambda e, bcs=bcs, pb_t=pb_t: e.copy(out=bcs[0:64, :], in_=pb_t[0:64, :]), reads=[b_pb],
                                 writes=[b_bcs])
                            k.op("dve", lambda e, og=og, po=po, bcs=bcs, off=off: e.tensor_tensor(
                                out=og[0:64, :, off:off + 128], in0=po[0:64, :].rearrange("p (s n) -> p s n", s=4),
                                in1=bcs[0:64, :].rearrange("p (s n) -> p s n", s=4), op=ALU.mult),
                                reads=[b_po, b_bcs], writes=[b_og])
                            if off + 128 == span:
                                for half in range(2):
                                    k.dma("sp", mixT_d[2 + 2 * g:4 + 2 * g, half * 64:(half + 1) * 64,
                                                       base * 128:base * 128 + span].rearrange("c p n -> p c n"),
                                          og[0:64, half * 2:(half + 1) * 2, 0:span], reads=[b_og])
                    run_pipelined(gen_swa, [(i, g) for i in blocks for g in range(2)], 2)
                    k.barrier()
            if stop_after == ("P2", layer):
                break

            with contextlib.ExitStack() as st:
                sb, ps = mk_alloc(st)
                Gs = sb("Gs", [128, NT, 256], BF16)
                Vc = sb("Vc", [128, NT, 256], BF16)
                Kd = sb("Kd", [128, NT, 2, 256], BF16)
                QKc = sb("QKc", [128, 6, NT * 128], BF16)
                Mk = sb("Mk", [128, 4, 128], F32)
                qdec = sb("qdec", [128, 8], F32)
                kdec = sb("kdec", [128, 8], F32)
                cdT = sb("cdT", [128, 2, 2, 64], F32)
                b_Gs, b_Vc, b_Kd, b_QKc, b_cst = Buf(), Buf(), Buf(), Buf(), Buf()
                with contextlib.ExitStack() as stw:
                    sbw, _ = mk_alloc(stw)
                    b_s = Buf()
                    lgR = sbw("lgR", [128, 8], F32)
                    lgP = sbw("lgP", [128, 4], F32)
                    for d_, nm in enumerate(("ret_decay_fwd", "ret_decay_bwd")):
                        k.dma("sp", lgR[:, d_ * 4:(d_ + 1) * 4], W[nm][layer:layer + 1, :].broadcast_to([128, 4]), writes=[b_s])
                        for two in range(2):
                            for pr in range(2):
                                hh = 2 * pr + two
                                k.dma("sp", lgP[two * 64:(two + 1) * 64, d_ * 2 + pr:d_ * 2 + pr + 1],
                                      W[nm][layer:layer + 1, hh:hh + 1].broadcast_to([64, 1]), writes=[b_s])
                    for tl in (lgR, lgP):
                        k.op("act", lambda e, tl=tl: e.activation(out=tl[:], in_=tl[:], func=AF.Exp, scale=-1.0), reads=[b_s],
                             writes=[b_s])
                        k.op("dve", lambda e, tl=tl: e.tensor_scalar(out=tl[:], in0=tl[:], scalar1=1.0, scalar2=None, op0=ALU.add),
                             reads=[b_s], writes=[b_s])
                        k.op("act", lambda e, tl=tl: e.activation(out=tl[:], in_=tl[:], func=AF.Ln), reads=[b_s], writes=[b_s])
                        k.op("dve", lambda e, tl=tl: e.tensor_scalar(out=tl[:], in0=tl[:], scalar1=-1.0, scalar2=None, op0=ALU.mult),
                             reads=[b_s], writes=[b_s])
                    cf = sbw("retc", [128, 6, 128], F32)
                    for i_, nm in enumerate(("ret_d1", "ret_i1", "ret_d2", "ret_i2", "ret_j1", "ret_j2")):
                        k.dma("sp", cf[:, i_, :], C[nm][:, :], writes=[b_s])
                    pc = sbw("retpc", [128, 4], F32)
                    k.dma("sp", pc[:], C["ret_pc"][:, :], writes=[b_s])
                    e1 = sbw("e1", [128, 128], F32)
                    e2 = sbw("e2", [128, 128], F32)
                    for h in range(4):
                        k.op("act", lambda e, h=h: e.activation(out=e1[:], in_=cf[:, 0, :], func=AF.Exp, scale=lgR[:, h:h + 1]),
                             reads=[b_s], writes=[b_s])
                        k.op("dve", lambda e: e.tensor_tensor(out=e1[:], in0=e1[:], in1=cf[:, 1, :], op=ALU.mult), reads=[b_s],
                             writes=[b_s])
                        k.op("act", lambda e, h=h: e.activation(out=e2[:], in_=cf[:, 2, :], func=AF.Exp, scale=lgR[:, 4 + h:5 + h]),
                             reads=[b_s], writes=[b_s])
                        k.op("dve", lambda e: e.tensor_tensor(out=e2[:], in0=e2[:], in1=cf[:, 3, :], op=ALU.mult), reads=[b_s],
                             writes=[b_s])
                        k.op("dve", lambda e, h=h: e.tensor_tensor(out=Mk[:, h, :], in0=e1[:], in1=e2[:], op=ALU.add), reads=[b_s],
                             writes=[b_cst])
                    k.op("dve", lambda e: e.tensor_scalar(out=qdec[:, 0:4], in0=lgR[:, 0:4], scalar1=pc[:, 2:3], scalar2=None,
                                                          op0=ALU.mult), reads=[b_s], writes=[b_cst])
                    k.op("dve", lambda e: e.tensor_scalar(out=qdec[:, 4:8], in0=lgR[:, 4:8], scalar1=pc[:, 3:4], scalar2=None,
                                                          op0=ALU.mult), reads=[b_s], writes=[b_cst])
                    k.op("act", lambda e: e.activation(out=qdec[:], in_=qdec[:], func=AF.Exp), reads=[b_cst], writes=[b_cst])
                    k.op("dve", lambda e: e.tensor_scalar(out=kdec[:, 0:4], in0=lgR[:, 0:4], scalar1=pc[:, 0:1], scalar2=None,
                                                          op0=ALU.mult), reads=[b_s], writes=[b_cst])
                    k.op("dve", lambda e: e.tensor_scalar(out=kdec[:, 4:8], in0=lgR[:, 4:8], scalar1=pc[:, 1:2], scalar2=None,
                                                          op0=ALU.mult), reads=[b_s], writes=[b_cst])
                    k.op("act", lambda e: e.activation(out=kdec[:], in_=kdec[:], func=AF.Exp), reads=[b_cst], writes=[b_cst])
                    k.op("act", lambda e: e.activation(out=lgP[:], in_=lgP[:], func=AF.Exp, scale=128.0), reads=[b_s], writes=[b_s])
                    k.op("dve", lambda e: e.tensor_copy(out=cdT[:].rearrange("p a b n -> p (a b) n"),
                                                        in_=lgP[:, 0:4].rearrange("p (c o) -> p c o", o=1).broadcast_to([128, 4, 64])),
                         reads=[b_s], writes=[b_cst])
                    k.barrier()
                with contextlib.ExitStack() as stp:
                    sbp, psp = mk_alloc(stp)
                    winC = sbp("winC", [128, 8, 1024], BF16)
                    b_w = Buf()
                    with contextlib.ExitStack() as stw:
                        sbw, _ = mk_alloc(stw)
                        b_s = Buf()
                        for hf in range(2):
                            stg = sbw("stgC%d" % hf, [128, 8, 512], F32)
                            k.dma("sp", stg[:], W["w_in"][layer, :, 1120 + hf * 512:1120 + (hf + 1) * 512].rearrange(
                                "(c p) n -> p c n", p=128), writes=[b_s])
                            k.op("pool" if hf else "dve", lambda e, stg=stg, hf=hf: e.tensor_copy(
                                out=winC[:, :, hf * 512:(hf + 1) * 512], in_=stg[:]), reads=[b_s], writes=[b_w])
                        k.barrier()
                    hTr = Ring(sbp, "hTc", 3, [128, 8, 128], BF16)
                    rtc = Ring(sbp, "rtc", 3, [128, 2, 2, 64], F32)
                    qkr = Ring(sbp, "qkc", 3, [128, 512], F32)
                    tmpr = Ring(sbp, "rtmpc", 4, [128, 4, 64], F32)
                    qcr = Ring(sbp, "qc", 3, [128, 2, 4, 64], BF16)
                    kzr = Ring(sbp, "kz", 3, [128, 2, 2, 128], BF16)
                    for kt_, kb_ in zip(kzr.tiles, kzr.bufs):
                        k.op("pool", lambda e, kt_=kt_: e.memset(kt_[:], 0.0), writes=[kb_])
                    p1r = Ring(psp, "pC1", 2, [128, 512], F32)
                    p2r = Ring(psp, "pC2", 2, [128, 512], F32)
                    pTr = Ring(psp, "pTc", 2, [128, 8, 128], BF16)
                    v5c = lambda a: a.rearrange("p h (g j e) -> p h g j e", g=1, j=2)
                    def gen3(t):
                        isc = t >= NLT
                        hT, b_hT = hTr.next()
                        k.dma("sp", hT[:].rearrange("p c n -> p (c n)"), hT_d[t], writes=[b_hT])
                        p1, b_p1 = p1r.next()
                        p2, b_p2 = p2r.next()
                        for kc in range(8):
                            k.op("pe", lambda e, kc=kc, p1=p1, hT=hT: e.matmul(p1[:], lhsT=hT[:, kc, :], rhs=winC[:, kc, 0:512],
                                                                               start=(kc == 0), stop=(kc == 7)),
                                 reads=[b_hT, b_w], writes=[b_p1])
                        for kc in range(8):
                            k.op("pe", lambda e, kc=kc, p2=p2, hT=hT: e.matmul(p2[:], lhsT=hT[:, kc, :], rhs=winC[:, kc, 512:1024],
                                                                               start=(kc == 0), stop=(kc == 7)),
                                 reads=[b_hT, b_w], writes=[b_p2])
                        yield
                        qk, b_qk = qkr.next()
                        k.op("act", lambda e, qk=qk, p1=p1: e.copy(out=qk[:], in_=p1[:]), reads=[b_p1], writes=[b_qk])
                        k.op("act", lambda e, p2=p2, t=t: e.activation(out=Gs[:, t, :], in_=p2[:, 256:512], func=AF.Silu),
                             reads=[b_p2], writes=[b_Gs])
                        k.op("act", lambda e, p2=p2, t=t: e.copy(out=Vc[:, t, :], in_=p2[:, 0:256]), reads=[b_p2], writes=[b_Vc])
                        qc, b_qc = qcr.next()
                        tmp, b_tmp = tmpr.next()
                        qk4 = qk[:].rearrange("p (a h n) -> p a h n", a=2, h=4)
                        if isc:
                            k.op("pool", lambda e, qc=qc, qk4=qk4: e.tensor_copy(out=qc[:, 0, :, :], in_=qk4[:, 0, :, :]),
                                 reads=[b_qk], writes=[b_qc])
                            k.op("pool", lambda e, qc=qc, qk4=qk4: e.tensor_scalar(out=qc[:, 1, :, :], in0=qk4[:, 1, :, :],
                                                                                   scalar1=0.125, scalar2=None, op0=ALU.mult),
                                 reads=[b_qk], writes=[b_qc])
                        else:
                            rt, b_rt = rtc.next()
                            k.dma("sp", rt[:, 0, :, :], C["ropeC_c"][:, t, :, :], writes=[b_rt])
                            k.dma("sp", rt[:, 1, :, :], C["ropeC_s"][:, t, :, :], writes=[b_rt])
                            for a_, eng in ((0, "dve"), (1, "pool")):
                                tc_ = v5c(rt[:, 0, a_:a_ + 1, :].broadcast_to([128, 4, 64]))
                                ts_ = v5c(rt[:, 1, a_:a_ + 1, :].broadcast_to([128, 4, 64]))
                                b_t2 = Buf()
                                tm = tmp if a_ == 0 else tmpr.next()[0]
                                rope(eng, v5c(qc[:, a_, :, :]), v5c(qk4[:, a_, :, :]), tc_, ts_, v5c(tm[:]), [b_qk, b_rt], [b_qc],
                                     b_tmp if a_ == 0 else tmpr.bufs[(tmpr.i - 1) % 4])
                        for d_ in range(2):
                            k.op("pool" if d_ else "dve", lambda e, qc=qc, d_=d_, t=t: e.tensor_tensor(
                                out=Kd[:, t, d_, :].rearrange("p (h n) -> p h n", h=4), in0=qc[:, 1, :, :],
                                in1=kdec[:, d_ * 4:(d_ + 1) * 4].rearrange("p (h o) -> p h o", o=1).broadcast_to([128, 4, 64]),
                                op=ALU.mult), reads=[b_qc, b_cst], writes=[b_Kd])
                        yield
                        kz, b_kz = kzr.next()
                        qc5 = qc[:, 1, :, :].rearrange("p (pr hl) n -> p pr hl n", hl=2)
                        for hl in range(2):
                            k.op("pool", lambda e, kz=kz, qc5=qc5, hl=hl: e.tensor_copy(
                                out=kz[:, :, hl, hl * 64:(hl + 1) * 64], in_=qc5[:, :, hl, :]), reads=[b_qc], writes=[b_kz])
                        pT, b_pT = pTr.next()
                        for pr in range(2):
                            k.op("pe", lambda e, pr=pr, pT=pT, qc=qc: e.transpose(
                                out=pT[:, pr, :], in_=qc[:, 0, 2 * pr:2 * pr + 2, :].rearrange("p h n -> p (h n)"),
                                identity=ident_b[:]), reads=[b_qc], writes=[b_pT])
                            for hl in range(2):
                                k.op("pe", lambda e, pr=pr, hl=hl, pT=pT, kz=kz: e.transpose(
                                    out=pT[:, 2 + 2 * pr + hl, :], in_=kz[:, pr, hl, :], identity=ident_b[:]),
                                    reads=[b_kz], writes=[b_pT])
                        k.op("act", lambda e, pT=pT, t=t: e.copy(out=QKc[:, :, t * 128:(t + 1) * 128], in_=pT[:, 0:6, :]),
                             reads=[b_pT], writes=[b_QKc])
                    run_pipelined(gen3, range(NT), 3)
                    k.barrier()
                if stop_after == ("P3a", layer):
                    break
                with contextlib.ExitStack() as stp:
                    sbp, psp = mk_alloc(stp)
                    Sst = sbp("Sst", [128, 2, 2, NT, 128], BF16)
                    b_Sst = Buf()
                    k.op("pool", lambda e: e.memset(Sst[:], 0.0), writes=[b_Sst])
                    pUr = Ring(psp, "pU", 1, [128, 512], F32)
                    for d_ in range(2):
                        order = [NLT, NLT + 1] + list(range(NLT)) if d_ == 0 else [NLT + 1, NLT] + list(range(NLT - 1, -1, -1))
                        S = sbp("S%d" % d_, [128, 2, 64], F32)
                        b_S = Buf()
                        k.op("pool", lambda e, S=S: e.memset(S[:], 0.0), writes=[b_S])
                        for t in order:
                            for hl in range(2):
                                k.op("act", lambda e, S=S, d_=d_, t=t, hl=hl: e.copy(
                                    out=Sst[hl * 64:(hl + 1) * 64, d_, :, t, hl * 64:(hl + 1) * 64], in_=S[hl * 64:(hl + 1) * 64, :, :]),
                                    reads=[b_S], writes=[b_Sst])
                            if t == order[-1]:
                                break
                            pU, b_pU = pUr.next()
                            for pr in range(2):
                                k.op("pe", lambda e, pU=pU, pr=pr, t=t, d_=d_: e.matmul(
                                    pU[:, pr * 128:(pr + 1) * 128], lhsT=Kd[:, t, d_, pr * 128:(pr + 1) * 128],
                                    rhs=Vc[:, t, pr * 128:(pr + 1) * 128], start=True, stop=True, skip_group_check=True),
                                    reads=[b_Kd, b_Vc], writes=[b_pU])
                            k.op("dve", lambda e, S=S, d_=d_: e.tensor_tensor(out=S[:], in0=S[:], in1=cdT[:, d_, :, :], op=ALU.mult),
                                 reads=[b_S, b_cst], writes=[b_S])
                            for hl in range(2):
                                k.op("dve", lambda e, S=S, pU=pU, hl=hl: e.tensor_tensor(
                                    out=S[hl * 64:(hl + 1) * 64, :, :], in0=S[hl * 64:(hl + 1) * 64, :, :],
                                    in1=pU[hl * 64:(hl + 1) * 64, 0:256].rearrange("p (a n) -> p a n", a=2)[:, :, hl * 64:(hl + 1) * 64],
                                    op=ALU.add), reads=[b_S, b_pU], writes=[b_S])
                    pAr = Ring(psp, "pAc", 2, [128, 512], F32)
                    pOr = Ring(psp, "pOc", 2, [128, 512], F32)
                    pQr = Ring(psp, "pQc", 2, [128, 512], F32)
                    oar = Ring(sbp, "oa", 2, [128, 512], F32)
                    pTr = Ring(psp, "pTo", 1, [128, 8, 128], BF16)
                    ATr = Ring(sbp, "AT", 2, [128, 512], BF16)
                    s4r = Ring(sbp, "s4c", 2, [128, 8], F32)
                    junk = sbp("junkc", [128, 64], F32)
                    b_junk = Buf()
                    o1r = Ring(sbp, "o1", 2, [128, 256], F32)
                    o2r = Ring(sbp, "o2", 2, [128, 256], BF16)
                    ostg = Ring(sbp, "ostc", 2, [128, 2, 512], BF16)
                    curd = {}

                    def gen_ro(t):
                        isc = t >= NLT
                        base, off, span = (NLT, (t - NLT) * 128, 256) if isc else ((t // 4) * 4, (t % 4) * 128, 512)
                        if off == 0:
                            curd[base] = ostg.next()
                        og, b_og = curd[base]
                        pA, b_pA = pAr.next()
                        for h in range(4):
                            pr, hl = h // 2, h % 2
                            k.op("pe", lambda e, pA=pA, h=h, pr=pr, hl=hl, t=t: e.matmul(
                                pA[:, h * 128:(h + 1) * 128], lhsT=QKc[:, 2 + 2 * pr + hl, t * 128:(t + 1) * 128],
                                rhs=QKc[:, pr, t * 128:(t + 1) * 128], start=True, stop=True,
                                skip_group_check=True), reads=[b_QKc], writes=[b_pA])
                        AT, b_AT = ATr.next()
                        k.op("dve", lambda e, AT=AT, pA=pA: e.tensor_tensor(out=AT[:], in0=pA[:],
                                                                            in1=Mk[:].rearrange("p h n -> p (h n)"), op=ALU.mult),
                             reads=[b_pA, b_cst], writes=[b_AT])
                        pO, b_pO = pOr.next()
                        pQ, b_pQ = pQr.next()
                        for h in range(4):
                            k.op("pe", lambda e, pO=pO, AT=AT, h=h, t=t: e.matmul(
                                pO[:, h * 64:(h + 1) * 64], lhsT=AT[:, h * 128:(h + 1) * 128], rhs=Vc[:, t, h * 64:(h + 1) * 64],
                                start=True, stop=True, skip_group_check=True), reads=[b_AT, b_Vc], writes=[b_pO])
                        for d_ in range(2):
                            for pr in range(2):
                                k.op("pe", lambda e, pQ=pQ, t=t, d_=d_, pr=pr: e.matmul(
                                    pQ[:, d_ * 256 + pr * 128:d_ * 256 + (pr + 1) * 128], lhsT=QKc[:, pr, t * 128:(t + 1) * 128],
                                    rhs=Sst[:, d_, pr, t, :], start=True, stop=True, skip_group_check=True),
                                    reads=[b_QKc, b_Sst], writes=[b_pQ])
                        yield
                        oa, b_oa = oar.next()
                        k.op("dve", lambda e, oa=oa, pQ=pQ: e.tensor_tensor(
                            out=oa[:].rearrange("p (a h n) -> p a h n", a=2, h=4),
                            in0=pQ[:].rearrange("p (a h n) -> p a h n", a=2, h=4),
                            in1=qdec[:].rearrange("p (a h o) -> p a h o", a=2, o=1).broadcast_to([128, 2, 4, 64]), op=ALU.mult),
                            reads=[b_pQ, b_cst], writes=[b_oa])
                        k.op("pool", lambda e, oa=oa: e.tensor_tensor(out=oa[:, 0:256], in0=oa[:, 0:256], in1=oa[:, 256:512], op=ALU.add),
                             reads=[b_oa], writes=[b_oa])
                        k.op("dve", lambda e, oa=oa, pO=pO: e.tensor_tensor(out=oa[:, 0:256], in0=pO[:, 0:256], in1=oa[:, 0:256], op=ALU.add),
                             reads=[b_pO, b_oa], writes=[b_oa])
                        pO, b_pO = oa, b_oa
                        s4, b_s4 = s4r.next()
                        for h in range(4):
                            k.op("act", lambda e, pO=pO, s4=s4, h=h: e.activation(out=junk[:], in_=pO[:, h * 64:(h + 1) * 64],
                                                                                 func=AF.Square, accum_out=s4[:, h:h + 1]),
                                 reads=[b_pO], writes=[b_junk, b_s4])
                        rms_rstd(s4[:, 0:4], s4[:, 4:8], 64, b_s4, b_s4)
                        o1, b_o1 = o1r.next()
                        o2, b_o2 = o2r.next()
                        k.op("dve", lambda e, o1=o1, pO=pO, s4=s4: e.tensor_tensor(
                            out=o1[:].rearrange("p (h n) -> p h n", h=4), in0=pO[:, 0:256].rearrange("p (h n) -> p h n", h=4),
                            in1=s4[:, 4:8].rearrange("p (h o) -> p h o", o=1).broadcast_to([128, 4, 64]), op=ALU.mult),
                            reads=[b_pO, b_s4], writes=[b_o1])
                        k.op("pool", lambda e, o1=o1, o2=o2, t=t: e.tensor_tensor(out=o2[:], in0=o1[:], in1=Gs[:, t, :], op=ALU.mult),
                             reads=[b_o1, b_Gs], writes=[b_o2])
                        pT, b_pT = pTr.next()
                        for pr in range(2):
                            k.op("pe", lambda e, pT=pT, o2=o2, pr=pr: e.transpose(out=pT[:, pr, :], in_=o2[:, pr * 128:(pr + 1) * 128],
                                                                                 identity=ident_b[:]), reads=[b_o2], writes=[b_pT])
                        k.op("act", lambda e, og=og, pT=pT, off=off: e.copy(out=og[:, :, off:off + 128], in_=pT[:, 0:2, :]),
                             reads=[b_pT], writes=[b_og])
                        if off + 128 == span:
                            k.dma("sp", mixT_d[6:8, :, base * 128:base * 128 + span].rearrange("c p n -> p c n"), og[:, :, 0:span],
                                  reads=[b_og])
                    run_pipelined(gen_ro, range(ntile), 2)
                    k.barrier()
            if stop_after == ("P3", layer):
                break

            b_xr = [Buf() for _ in range(NT)]
            with contextlib.ExitStack() as st:
                sb, ps = mk_alloc(st)
                wo = [sb("woL", [128, 8, D], BF16), None if last else sb("woC", [128, 8, D], BF16)]
                b_w = Buf()
                with contextlib.ExitStack() as stw:
                    sbw, _ = mk_alloc(stw)
                    b_s = Buf()
                    g1 = [sbw("g1L", [128, D], F32), sbw("g1C", [128, D], F32)]
                    for wh in range(1 if last else 2):
                        load_rep(g1[wh][:], mod_row(wh, 2), b_s)
                    for hf in range(2):
                        stg = sbw("stgO%d" % hf, [128, 8, 512], F32)
                        k.dma("sp", stg[:], W["w_out"][layer, :, hf * 512:(hf + 1) * 512].rearrange("(c p) n -> p c n", p=128),
                              writes=[b_s])
                        for wh in range(1 if last else 2):
                            k.op("pool" if wh else "dve", lambda e, stg=stg, hf=hf, wh=wh: e.tensor_tensor(
                                out=wo[wh][:, :, hf * 512:(hf + 1) * 512], in0=stg[:],
                                in1=g1[wh][:, hf * 512:(hf + 1) * 512].rearrange("p (o n) -> p o n", o=1).broadcast_to([128, 8, 512]),
                                op=ALU.mult), reads=[b_s], writes=[b_w])
                    k.barrier()
                mTr = Ring(sb, "mT", 2, [128, 8, 512], BF16)
                xr = Ring(sb, "xt4", 3, [128, D], F32)
                pYr = Ring(ps, "pY4", 4, [128, 512], F32)
                for t0 in range(0, ntile, 4):
                    nt_ = min(4, ntile - t0)
                    mT, b_mT = mTr.next()
                    k.dma("sp", mT[:, :, 0:nt_ * 128], mixT_d[:, :, t0 * 128:(t0 + nt_) * 128].rearrange("c p n -> p c n"),
                          writes=[b_mT])
                    for j in range(nt_):
                        t = t0 + j
                        wsel = wo[1] if t >= NLT else wo[0]
                        xt, b_x = xr.next()
                        k.dma("sp", xt[:], xres[t * 128:(t + 1) * 128, :], reads=[b_xr[t]], writes=[b_x])
                        for hf in range(2):
                            pY, b_pY = pYr.next()
                            for c in range(8):
                                k.op("pe", lambda e, pY=pY, mT=mT, c=c, j=j, hf=hf, wsel=wsel: e.matmul(
                                    pY[:], lhsT=mT[:, c, j * 128:(j + 1) * 128], rhs=wsel[:, c, hf * 512:(hf + 1) * 512],
                                    start=(c == 0), stop=(c == 7)), reads=[b_mT, b_w], writes=[b_pY])
                            k.op("dve", lambda e, xt=xt, pY=pY, hf=hf: e.tensor_tensor(
                                out=xt[:, hf * 512:(hf + 1) * 512], in0=pY[:], in1=xt[:, hf * 512:(hf + 1) * 512], op=ALU.add),
                                reads=[b_pY, b_x], writes=[b_x])
                        k.dma("act", xres[t * 128:(t + 1) * 128, :], xt[:], reads=[b_x], writes=[b_xr[t]])
                k.barrier()
            if stop_after == ("P4", layer):
                break

            moe = (layer % 2 == 1)
            with contextlib.ExitStack() as st:
                sb, ps = mk_alloc(st)
                if moe:
                    nexp, nch = NE, D_FFE // 128
                    wsrc = lambda e_: (W["moe_w_gate"][0, e_], W["moe_w_up"][0, e_], W["moe_w_down"][0, e_])
                else:
                    nexp, nch = 1, D_FF // 128
                    wsrc = lambda e_: (W["ffn_w_gate"][0], W["ffn_w_up"][0], W["ffn_w_down"][0])
                FB = 4
                fblocks = [(c0, min(FB, nch - c0)) for c0 in range(0, nch, FB)]
                tblocks = [(t0, 8, 0) for t0 in range(0, NLT, 8)] + ([] if last else [(NLT, 2, 1)])
                acc = sb("acc", [128, 8, D], F32)
                h2T = sb("h2T", [128, 8, 1024], BF16)
                gates = sb("gates", [128, 8, 8], F32)
                b_acc = [Buf() for _ in range(8)]
                b_h2T, b_gates = Buf(), Buf()
                wgr = Ring(sb, "wg", 2, [128, 8, FB * 128], BF16)
                wur = Ring(sb, "wu", 2, [128, 8, FB * 128], BF16)
                wdr = Ring(sb, "wd", 2, [128, FB, D], BF16)
                stgr = Ring(sb, "stgF", 3, [128, 4096], F32)
                actr = Ring(sb, "actT", 2, [128, FB, 512], BF16)
                sgr = Ring(sb, "sg", 2, [128, 512], BF16)
                h2r = Ring(sb, "h2f", 3, [128, D], F32)
                hbr = Ring(sb, "h2b", 3, [128, D], BF16)
                s4r = Ring(sb, "s4f", 4, [128, 8], F32)
                junk = sb("junkf", [128, D], BF16)
                b_junk = Buf()
                psG = Ring(ps, "psG", 2, [128, 512], F32)
                psU = Ring(ps, "psU", 2, [128, 512], F32)
                psY = Ring(ps, "psY", 2, [128, 512], F32)
                psT = Ring(ps, "psT", 1, [128, 8, 128], BF16)
                psR = Ring(ps, "psR", 1, [128, 4, 128], F32)
                if moe:
                    rtf = sb("rtf", [128, 8, 8], F32)
                    h2Tf = sb("h2Tf", [128, 8, 128], F32)
                    b_rtf, b_h2Tf = Buf(), Buf()
                    k.dma("sp", rtf[:], W["moe_router"][0].rearrange("(c p) n -> p c n", p=128), writes=[b_rtf])
                    g8 = Ring(sb, "g8", 3, [128, 32], F32)
                if last:
                    gF = sb("gF", [128, D], F32)
                    b_gF = Buf()
                    load_rep(gF[:], W["final_norm_g"].rearrange("(o n) -> o n", o=1), b_gF)
                    outr = Ring(sb, "outt", 2, [128, D], F32)
                aff = {}
                for wh in sorted(set(tb[2] for tb in tblocks)):
                    A2, S2, b_af = load_affine(sb, "n2%d" % wh, wh, W["norm2_g"], 4, 3)
                    G2 = sb("g2r%d" % wh, [128, D], F32)
                    load_rep(G2[:], mod_row(wh, 5), b_af)
                    aff[wh] = (A2, S2, G2, b_af)
                k.barrier()
                for (t0, ntb, wh) in tblocks:
                    A2, S2, G2, b_af = aff[wh]

                    def gen_pro(j, t0=t0, A2=A2, S2=S2, b_af=b_af):
                        t = t0 + j
                        k.dma("sp", acc[:, j, :], xres[t * 128:(t + 1) * 128, :], reads=[b_xr[t]], writes=[b_acc[j]])
                        s4, b_s4 = s4r.next()
                        k.op("act", lambda e, j=j, s4=s4: e.activation(out=junk[:], in_=acc[:, j, :], func=AF.Square,
                                                                       accum_out=s4[:, 0:1]), reads=[b_acc[j]], writes=[b_junk, b_s4])
                        rms_rstd(s4[:, 0:1], s4[:, 1:2], D, b_s4, b_s4)
                        h2, b_h2 = h2r.next()
                        hb, b_hb = hbr.next()
                        k.op("dve", lambda e, j=j, s4=s4, h2=h2, A2=A2: e.scalar_tensor_tensor(
                            out=h2[:], in0=acc[:, j, :], scalar=s4[:, 1:2], in1=A2[:], op0=ALU.mult, op1=ALU.mult),
                            reads=[b_acc[j], b_s4, b_af], writes=[b_h2])
                        k.op("pool", lambda e, h2=h2, S2=S2: e.tensor_tensor(out=h2[:], in0=h2[:], in1=S2[:], op=ALU.add),
                             reads=[b_h2, b_af], writes=[b_h2])
                        k.op("pool", lambda e, h2=h2, hb=hb: e.tensor_copy(out=hb[:], in_=h2[:]), reads=[b_h2], writes=[b_hb])
                        yield
                        pT, b_pT = psT.next()
                        for kc in range(8):
                            k.op("pe", lambda e, kc=kc, pT=pT, hb=hb: e.transpose(out=pT[:, kc, :], in_=hb[:, kc * 128:(kc + 1) * 128],
                                                                                  identity=ident_b[:]), reads=[b_hb], writes=[b_pT])
                        k.op("act", lambda e, pT=pT, j=j: e.copy(out=h2T[:, :, j * 128:(j + 1) * 128], in_=pT[:]), reads=[b_pT],
                             writes=[b_h2T])
                        if moe:
                            for r_ in range(2):
                                pR, b_pR = psR.next()
                                for c4 in range(4):
                                    kc = r_ * 4 + c4
                                    k.op("pe", lambda e, pR=pR, c4=c4, kc=kc, h2=h2: e.transpose(
                                        out=pR[:, c4, :], in_=h2[:, kc * 128:(kc + 1) * 128], identity=ident_f[:]),
                                        reads=[b_h2], writes=[b_pR])
                                k.op("act", lambda e, pR=pR, r_=r_: e.copy(out=h2Tf[:, r_ * 4:(r_ + 1) * 4, :], in_=pR[:]),
                                     reads=[b_pR], writes=[b_h2Tf])
                            yield
                            pL, b_pL = psY.next()
                            for kc in range(8):
                                k.op("pe", lambda e, pL=pL, kc=kc: e.matmul(pL[:, 0:8], lhsT=h2Tf[:, kc, :], rhs=rtf[:, kc, :],
                                                                            start=(kc == 0), stop=(kc == 7)),
                                     reads=[b_h2Tf, b_rtf], writes=[b_pL])
                            g, b_g = g8.next()
                            k.op("dve", lambda e, g=g, pL=pL: e.tensor_copy(out=g[:, 0:8], in_=pL[:, 0:8]), reads=[b_pL], writes=[b_g])
                            k.op("dve", lambda e, g=g: e.max(out=g[:, 8:16], in_=g[:, 0:8]), reads=[b_g], writes=[b_g])
                            k.op("dve", lambda e, g=g: e.tensor_scalar(out=g[:, 16:24], in0=g[:, 0:8], scalar1=g[:, 8:9], scalar2=None,
                                                                       op0=ALU.subtract), reads=[b_g], writes=[b_g])
                            k.op("act", lambda e, g=g: e.activation(out=g[:, 16:24], in_=g[:, 16:24], func=AF.Exp), reads=[b_g],
                                 writes=[b_g])
                            k.op("dve", lambda e, g=g: e.scalar_tensor_tensor(out=g[:, 16:24], in0=g[:, 0:8], scalar=g[:, 9:10],
                                                                              in1=g[:, 16:24], op0=ALU.is_ge, op1=ALU.mult),
                                 reads=[b_g], writes=[b_g])
                            k.op("dve", lambda e, g=g: e.reduce_sum(out=g[:, 24:25], in_=g[:, 16:24], axis=AX.X), reads=[b_g],
                                 writes=[b_g])
                            k.op("dve", lambda e, g=g: e.reciprocal(out=g[:, 24:25], in_=g[:, 24:25]), reads=[b_g], writes=[b_g])
                            k.op("dve", lambda e, g=g, j=j: e.tensor_scalar(out=gates[:, j, :], in0=g[:, 16:24], scalar1=g[:, 24:25],
                                                                            scalar2=None, op0=ALU.mult), reads=[b_g],
                                 writes=[b_gates])
                    run_pipelined(gen_pro, range(ntb), 3)
                    subs = [(s0, min(4, ntb - s0)) for s0 in range(0, ntb, 4)]
                    for e_ in range(nexp):
                        wg_d, wu_d, wd_d = wsrc(e_)
                        for (c0, F) in fblocks:
                            wg, b_wg = wgr.next()
                            wu, b_wu = wur.next()
                            wd, b_wd = wdr.next()
                            for (dst, b_dst, srcw, eng) in ((wg, b_wg, wg_d, "pool"), (wu, b_wu, wu_d, "act")):
                                sg_, b_sg_ = stgr.next()
                                sv_ = sg_[:, 0:8 * F * 128].rearrange("p (c n) -> p c n", c=8)
                                k.dma("sp", sv_, srcw[:, c0 * 128:(c0 + F) * 128].rearrange("(c p) n -> p c n", p=128),
                                      writes=[b_sg_])
                                if eng == "pool":
                                    k.op("pool", lambda e, dst=dst, sv_=sv_, F=F: e.tensor_copy(out=dst[:, :, 0:F * 128], in_=sv_),
                                         reads=[b_sg_], writes=[b_dst])
                                else:
                                    k.op("act", lambda e, dst=dst, sv_=sv_, F=F: e.copy(out=dst[:, :, 0:F * 128], in_=sv_),
                                         reads=[b_sg_], writes=[b_dst])
                            sg_, b_sg_ = stgr.next()
                            sv_ = sg_[:, 0:F * D].rearrange("p (c n) -> p c n", c=F)
                            k.dma("sp", sv_, wd_d[c0 * 128:(c0 + F) * 128, :].rearrange("(c p) n -> p c n", p=128), writes=[b_sg_])
                            k.op("pool", lambda e, wd=wd, sv_=sv_, F=F, G2=G2: e.tensor_tensor(
                                out=wd[:, 0:F, :], in0=sv_, in1=G2[:].rearrange("p (o n) -> p o n", o=1).broadcast_to([128, F, D]),
                                op=ALU.mult), reads=[b_sg_, b_af], writes=[b_wd])
                            for (s0, ns) in subs:
                                N = ns * 128
                                aT, b_aT = actr.next()
                                for fc in range(F):
                                    pG, b_pG = psG.next()
                                    pU_, b_pU = psU.next()
                                    for kc in range(8):
                                        k.op("pe", lambda e, pG=pG, wg=wg, kc=kc, fc=fc, s0=s0, N=N: e.matmul(
                                            pG[:, 0:N], lhsT=wg[:, kc, fc * 128:(fc + 1) * 128], rhs=h2T[:, kc, s0 * 128:s0 * 128 + N],
                                            start=(kc == 0), stop=(kc == 7)), reads=[b_wg, b_h2T], writes=[b_pG])
                                    for kc in range(8):
                                        k.op("pe", lambda e, pU_=pU_, wu=wu, kc=kc, fc=fc, s0=s0, N=N: e.matmul(
                                            pU_[:, 0:N], lhsT=wu[:, kc, fc * 128:(fc + 1) * 128], rhs=h2T[:, kc, s0 * 128:s0 * 128 + N],
                                            start=(kc == 0), stop=(kc == 7)), reads=[b_wu, b_h2T], writes=[b_pU])
                                    sgt, b_sgt = sgr.next()
                                    k.op("act", lambda e, sgt=sgt, pG=pG, N=N: e.activation(out=sgt[:, 0:N], in_=pG[:, 0:N], func=AF.Silu),
                                         reads=[b_pG], writes=[b_sgt])
                                    k.op("dve", lambda e, aT=aT, fc=fc, sgt=sgt, pU_=pU_, N=N: e.tensor_tensor(
                                        out=aT[:, fc, 0:N], in0=pU_[:, 0:N], in1=sgt[:, 0:N], op=ALU.mult),
                                        reads=[b_pU, b_sgt], writes=[b_aT])
                                for jj in range(ns):
                                    j = s0 + jj
                                    for hf in range(2):
                                        pY, b_pY = psY.next()
                                        for fc in range(F):
                                            k.op("pe", lambda e, pY=pY, aT=aT, wd=wd, fc=fc, jj=jj, hf=hf, F=F: e.matmul(
                                                pY[:], lhsT=aT[:, fc, jj * 128:(jj + 1) * 128], rhs=wd[:, fc, hf * 512:(hf + 1) * 512],
                                                start=(fc == 0), stop=(fc == F - 1)), reads=[b_aT, b_wd], writes=[b_pY])
                                        if moe:
                                            k.op("dve", lambda e, pY=pY, j=j, hf=hf, e_=e_: e.scalar_tensor_tensor(
                                                out=acc[:, j, hf * 512:(hf + 1) * 512], in0=pY[:], scalar=gates[:, j, e_:e_ + 1],
                                                in1=acc[:, j, hf * 512:(hf + 1) * 512], op0=ALU.mult, op1=ALU.add),
                                                reads=[b_pY, b_gates, b_acc[j]], writes=[b_acc[j]])
                                        else:
                                            k.op("dve", lambda e, pY=pY, j=j, hf=hf: e.tensor_tensor(
                                                out=acc[:, j, hf * 512:(hf + 1) * 512], in0=pY[:],
                                                in1=acc[:, j, hf * 512:(hf + 1) * 512], op=ALU.add),
                                                reads=[b_pY, b_acc[j]], writes=[b_acc[j]])
                    for j in range(ntb):
                        t = t0 + j
                        if not last:
                            k.dma("act", xres[t * 128:(t + 1) * 128, :], acc[:, j, :], reads=[b_acc[j]], writes=[b_xr[t]])
                        else:
                            s4, b_s4 = s4r.next()
                            k.op("act", lambda e, j=j, s4=s4: e.activation(out=junk[:], in_=acc[:, j, :], func=AF.Square,
                                                                           accum_out=s4[:, 0:1]),
                                 reads=[b_acc[j]], writes=[b_junk, b_s4])
                            rms_rstd(s4[:, 0:1], s4[:, 1:2], D, b_s4, b_s4)
                            ot, b_ot = outr.next()
                            k.op("dve", lambda e, j=j, s4=s4, ot=ot: e.scalar_tensor_tensor(
                                out=ot[:], in0=acc[:, j, :], scalar=s4[:, 1:2], in1=gF[:], op0=ALU.mult, op1=ALU.mult),
                                reads=[b_acc[j], b_s4, b_gF], writes=[b_ot])
                            k.dma("act", out_d[t * 128:(t + 1) * 128, :], ot[:], reads=[b_ot])
                k.barrier()
            if stop_after == ("P5", layer):
                break
        k.barrier()
        k.emit()
    return nc, k


_CONSTS = None


def _in_maps(inputs):
    global _CONSTS
    if _CONSTS is None:
        _CONSTS = _consts()
    maps = []
    shared = {kk: np.ascontiguousarray(np.asarray(inputs[kk], dtype=np.float32)) for kk in W_SHAPES}
    for b in range(8):
        m = dict(shared)
        m.update(_CONSTS)
        m["x"] = np.ascontiguousarray(np.asarray(inputs["x"][b], dtype=np.float32))
        m["ctx"] = np.ascontiguousarray(np.asarray(inputs["ctx"][b], dtype=np.float32))
        m["c2"] = np.ascontiguousarray(np.stack([np.asarray(inputs["c"][b]), np.asarray(inputs["c_ctx"])]).astype(np.float32))
        maps.append(m)
    return maps


def kernel(**inputs):
    nc, _ = build()
    maps = _in_maps(inputs)
    res = run_bass_kernel_spmd(nc, maps, core_ids=list(range(8)))
    return np.stack([r["out"] for r in res.results], axis=0).astype(np.float32)
```
